# Optimizing a Trainium2 kernel written in Bass

```python
import math
import jax, jax.numpy as jnp
from jax import lax
import numpy as np

D_MODEL = 1024
BATCH = 8
SEQ = 4096
DEPTH = 2

D_MIX = D_MODEL
ML_HEADS = 4
ML_DH = 96
ML_W = ML_HEADS * ML_DH
ML_CHUNK = 64
ML_CONV = 4
SB_HEADS = 6
SB_DH = 64
SB_W = SB_HEADS * SB_DH
SB_BLOCK = 128
S5_GROUP_CH = 16
S5_W = D_MIX - ML_W - SB_W
S5_GROUPS = S5_W // S5_GROUP_CH
S5_STATE = 64
S5_DT_MIN = 1e-3
S5_DT_MAX = 1e-1
D_FF = 2816
FFN_CONV = 3
EPS = 1e-6
IN_WIDTHS = (2 * ML_W, ML_W, ML_W, 2 * ML_HEADS, SB_W, SB_W, SB_W, S5_W)
N_IN = 4 * ML_W + 2 * ML_HEADS + 3 * SB_W + S5_W

kernel_name = 'hymba_style_mlstm_stickbreak_s5_hybrid'


def rmsnorm(x, g):
    xf = x.astype(jnp.float32)
    y = xf * lax.rsqrt(jnp.mean(xf * xf, axis=-1, keepdims=True) + EPS)
    return (y * g.astype(jnp.float32)).astype(x.dtype)


def causal_dwconv(x, w):
    K, C = w.shape
    return lax.conv_general_dilated(
        x, w[:, None, :].astype(x.dtype), window_strides=(1,), padding=[(K - 1, 0)],
        dimension_numbers=('NWC', 'WIO', 'NWC'), feature_group_count=C)


def split_cols(z, widths):
    out, off = [], 0
    for w in widths:
        out.append(z[..., off:off + w])
        off += w
    return out


def mlstm(q, k, v, i_pre, f_pre):
    B, L, H, Dh = q.shape
    C = ML_CHUNK
    NC = L // C
    k = k * (Dh ** -0.5)
    qc = q.reshape(B, NC, C, H, Dh)
    kc = k.reshape(B, NC, C, H, Dh)
    vc = v.reshape(B, NC, C, H, Dh)
    ic = i_pre.reshape(B, NC, C, H)
    b = jnp.cumsum(jax.nn.log_sigmoid(f_pre).reshape(B, NC, C, H), axis=2)
    b_tot = b[:, :, -1]
    w_log = b_tot[:, :, None] - b + ic
    m_loc = jnp.max(w_log, axis=2)
    wexp = jnp.exp(w_log - m_loc[:, :, None])
    dC = jnp.einsum('bnchk,bnchv->bnhkv', wexp[..., None] * kc, vc)
    dn = jnp.einsum('bnch,bnchk->bnhk', wexp, kc)

    def step(carry, inp):
        Cs, ns, m = carry
        dC_j, dn_j, m_j, bt_j = inp
        m_new = jnp.maximum(bt_j + m, m_j)
        a = jnp.exp(bt_j + m - m_new)
        c = jnp.exp(m_j - m_new)
        Cn = a[..., None, None] * Cs + c[..., None, None] * dC_j
        nn = a[..., None] * ns + c[..., None] * dn_j
        return (Cn, nn, m_new), (Cs, ns, m)

    init = (jnp.zeros((B, H, Dh, Dh), jnp.float32), jnp.zeros((B, H, Dh), jnp.float32),
            jnp.zeros((B, H), jnp.float32))
    xs = (jnp.moveaxis(dC, 1, 0), jnp.moveaxis(dn, 1, 0), jnp.moveaxis(m_loc, 1, 0), jnp.moveaxis(b_tot, 1, 0))
    _, (C_prev, n_prev, m_prev) = lax.scan(step, init, xs)
    C_prev = jnp.moveaxis(C_prev, 0, 1)
    n_prev = jnp.moveaxis(n_prev, 0, 1)
    m_prev = jnp.moveaxis(m_prev, 0, 1)

    inter_log = b + m_prev[:, :, None]
    Dlog = b[:, :, :, None, :] - b[:, :, None, :, :] + ic[:, :, None, :, :]
    causal = jnp.tril(jnp.ones((C, C), dtype=bool))[None, None, :, :, None]
    Dlog = jnp.where(causal, Dlog, -jnp.inf)
    m_t = jnp.maximum(inter_log, jnp.max(Dlog, axis=3))
    s = jnp.einsum('bnthd,bnshd->bntsh', qc, kc) * jnp.exp(Dlog - m_t[:, :, :, None, :])
    inter_scale = jnp.exp(inter_log - m_t)
    num = (jnp.einsum('bntsh,bnshd->bnthd', s, vc)
           + inter_scale[..., None] * jnp.einsum('bnthk,bnhkv->bnthv', qc, C_prev))
    den = jnp.sum(s, axis=3) + inter_scale * jnp.einsum('bnthk,bnhk->bnth', qc, n_prev)
    h = num / jnp.maximum(jnp.abs(den), jnp.exp(-m_t))[..., None]
    return h.reshape(B, L, H, Dh)


def stick_breaking(q, k, v):
    B, L, H, Dh = q.shape
    scale = Dh ** -0.5
    outs = []
    for blk in range(L // SB_BLOCK):
        q0 = blk * SB_BLOCK
        kv_len = q0 + SB_BLOCK
        z = jnp.einsum('bthd,bshd->bhts', q[:, q0:kv_len], k[:, :kv_len]) * scale
        t_pos = q0 + jnp.arange(SB_BLOCK)
        s_pos = jnp.arange(kv_len)
        causal = s_pos[None, :] < t_pos[:, None]
        log_1mb = jnp.where(causal, jax.nn.log_sigmoid(-z), 0.0)
        suffix = lax.cumsum(log_1mb, axis=3, reverse=True) - log_1mb
        a = jnp.where(causal, jnp.exp(jax.nn.log_sigmoid(z) + suffix), 0.0)
        outs.append(jnp.einsum('bhts,bshd->bthd', a, v[:, :kv_len]))
    return jnp.concatenate(outs, axis=1)


def s5(u, a_re, a_im, log_dt, b_re, b_im, c_re, c_im, d, glu_w, glu_b):
    f32 = jnp.float32
    Bsz, L, _ = u.shape
    ug = u.reshape(Bsz, L, S5_GROUPS, S5_GROUP_CH)
    lam = lax.complex(jnp.minimum(a_re.astype(f32), -1e-4), a_im.astype(f32))
    dt = jnp.exp(log_dt.astype(f32))[:, None]
    lam_bar = jnp.exp(lam * dt)
    b_bar = ((lam_bar - 1.0) / lam)[..., None] * lax.complex(b_re.astype(f32), b_im.astype(f32))
    bu = jnp.einsum('gph,blgh->blgp', b_bar, ug.astype(jnp.complex64))
    a_full = jnp.broadcast_to(lam_bar, bu.shape)

    def combine(e1, e2):
        a1, b1 = e1
        a2, b2 = e2
        return a1 * a2, a2 * b1 + b2

    _, states = lax.associative_scan(combine, (a_full, bu), axis=1)
    y = jnp.einsum('ghp,blgp->blgh', lax.complex(c_re.astype(f32), c_im.astype(f32)), states).real
    y = y + d.astype(f32) * ug
    y = jax.nn.gelu(y.reshape(Bsz, L, S5_W))
    return y * jax.nn.sigmoid(y @ glu_w.astype(f32) + glu_b.astype(f32))


def setup_inputs(seed: int = 0) -> dict:
    key = jax.random.key(seed)
    ks = jax.random.split(key, 32)
    f32 = jnp.float32
    nrm = lambda k, shape: jax.random.normal(k, shape, f32)
    gain = lambda k, shape: 1.0 + 0.02 * nrm(k, shape)
    L_ = DEPTH
    f_bias = jnp.linspace(3.0, 6.0, ML_HEADS, dtype=f32)
    ml_gate_b = jnp.concatenate([0.1 * nrm(ks[5], (L_, ML_HEADS)),
                                 f_bias[None, :] + 0.1 * nrm(ks[6], (L_, ML_HEADS))], axis=-1)
    a_im0 = math.pi * jnp.arange(S5_STATE, dtype=f32)
    return {
        'x': nrm(ks[0], (BATCH, SEQ, D_MODEL)),
        'norm_mix_g': gain(ks[1], (L_, D_MODEL)),
        'w_in': nrm(ks[2], (L_, D_MODEL, N_IN)) * D_MODEL ** -0.5,
        'ml_conv_w': nrm(ks[3], (L_, ML_CONV, 2 * ML_W)) * ML_CONV ** -0.5,
        'ml_conv_b': 0.02 * nrm(ks[4], (L_, 2 * ML_W)),
        'ml_gate_b': ml_gate_b,
        'ml_out_g': gain(ks[7], (L_, ML_HEADS, ML_DH)),
        'sb_q_g': gain(ks[8], (L_, SB_DH)),
        'sb_k_g': gain(ks[9], (L_, SB_DH)),
        'sb_out_g': gain(ks[10], (L_, SB_HEADS, SB_DH)),
        's5_a_re': -0.5 + 0.01 * nrm(ks[11], (L_, S5_GROUPS, S5_STATE)),
        's5_a_im': a_im0 + 0.01 * nrm(ks[12], (L_, S5_GROUPS, S5_STATE)),
        's5_log_dt': jax.random.uniform(ks[13], (L_, S5_GROUPS), f32, math.log(S5_DT_MIN), math.log(S5_DT_MAX)),
        's5_b_re': nrm(ks[14], (L_, S5_GROUPS, S5_STATE, S5_GROUP_CH)) * (2 * S5_GROUP_CH) ** -0.5,
        's5_b_im': nrm(ks[15], (L_, S5_GROUPS, S5_STATE, S5_GROUP_CH)) * (2 * S5_GROUP_CH) ** -0.5,
        's5_c_re': nrm(ks[16], (L_, S5_GROUPS, S5_GROUP_CH, S5_STATE)) * (2 * S5_STATE) ** -0.5,
        's5_c_im': nrm(ks[17], (L_, S5_GROUPS, S5_GROUP_CH, S5_STATE)) * (2 * S5_STATE) ** -0.5,
        's5_d': nrm(ks[18], (L_, S5_GROUPS, S5_GROUP_CH)),
        's5_glu_w': nrm(ks[19], (L_, S5_W, S5_W)) * S5_W ** -0.5,
        's5_glu_b': 0.02 * nrm(ks[20], (L_, S5_W)),
        's5_out_g': gain(ks[21], (L_, S5_W)),
        'w_out': nrm(ks[22], (L_, D_MIX, D_MODEL)) * D_MIX ** -0.5,
        'norm_ffn_g': gain(ks[23], (L_, D_MODEL)),
        'ffn_w_up': nrm(ks[24], (L_, D_MODEL, 2 * D_FF)) * D_MODEL ** -0.5,
        'ffn_conv_w': nrm(ks[25], (L_, FFN_CONV, D_FF)) * FFN_CONV ** -0.5,
        'ffn_w_down': nrm(ks[26], (L_, D_FF, D_MODEL)) * D_FF ** -0.5,
    }


def reference(x, norm_mix_g, w_in, ml_conv_w, ml_conv_b, ml_gate_b, ml_out_g, sb_q_g, sb_k_g, sb_out_g,
              s5_a_re, s5_a_im, s5_log_dt, s5_b_re, s5_b_im, s5_c_re, s5_c_im, s5_d, s5_glu_w, s5_glu_b,
              s5_out_g, w_out, norm_ffn_g, ffn_w_up, ffn_conv_w, ffn_w_down):
    f32 = jnp.float32
    Bsz, L, _ = x.shape
    dtype = x.dtype
    for l in range(DEPTH):
        h = rmsnorm(x, norm_mix_g[l])
        z = h @ w_in[l]
        ml_qk, ml_v, ml_o, ml_if, sb_q, sb_k, sb_v, s5_u = split_cols(z, IN_WIDTHS)

        qk = jax.nn.silu(causal_dwconv(ml_qk, ml_conv_w[l]) + ml_conv_b[l])
        mq, mk = qk[..., :ML_W], qk[..., ML_W:]
        gates = (ml_if + ml_gate_b[l]).astype(f32)
        hm = mlstm(mq.reshape(Bsz, L, ML_HEADS, ML_DH).astype(f32),
                   mk.reshape(Bsz, L, ML_HEADS, ML_DH).astype(f32),
                   ml_v.reshape(Bsz, L, ML_HEADS, ML_DH).astype(f32),
                   gates[..., :ML_HEADS], gates[..., ML_HEADS:])
        hm = rmsnorm(hm, ml_out_g[l]).reshape(Bsz, L, ML_W) * jax.nn.sigmoid(ml_o.astype(f32))

        sq = rmsnorm(sb_q.reshape(Bsz, L, SB_HEADS, SB_DH).astype(f32), sb_q_g[l])
        sk = rmsnorm(sb_k.reshape(Bsz, L, SB_HEADS, SB_DH).astype(f32), sb_k_g[l])
        hs = stick_breaking(sq, sk, sb_v.reshape(Bsz, L, SB_HEADS, SB_DH).astype(f32))
        hs = rmsnorm(hs, sb_out_g[l]).reshape(Bsz, L, SB_W)

        h5 = s5(s5_u.astype(f32), s5_a_re[l], s5_a_im[l], s5_log_dt[l], s5_b_re[l], s5_b_im[l],
                s5_c_re[l], s5_c_im[l], s5_d[l], s5_glu_w[l], s5_glu_b[l])
        h5 = rmsnorm(h5, s5_out_g[l])

        mix = jnp.concatenate([hm, hs, h5], axis=-1).astype(dtype)
        x = x + mix @ w_out[l]

        h = rmsnorm(x, norm_ffn_g[l])
        up = h @ ffn_w_up[l]
        g, v = up[..., :D_FF], up[..., D_FF:]
        g = causal_dwconv(g, ffn_conv_w[l])
        x = x + (jax.nn.silu(g) * v) @ ffn_w_down[l]
    return x
```

```python
import math
import numpy as np
from contextlib import ExitStack
import concourse.bass as bass
import concourse.mybir as mybir
from concourse.bass_utils import run_bass_kernel_spmd

F32 = mybir.dt.float32
BF16 = mybir.dt.bfloat16
I32 = mybir.dt.int32
AF = mybir.ActivationFunctionType
ALU = mybir.AluOpType

D = 1024
NIN = 2952
DFF = 2816
TG = 512
EPS = 1e-6
NB_UP = 44
NKD = 22
TWO_PI = 2.0 * math.pi

IN_BLOCKS = []
for b in range(3):
    IN_BLOCKS.append(('v', b, 768 + 128 * b, 128))
IN_BLOCKS.append(('if', 0, 1536, 8))
for h in range(4):
    IN_BLOCKS.append(('q', h, 96 * h, 96))
    IN_BLOCKS.append(('k', h, 384 + 96 * h, 96))
    IN_BLOCKS.append(('o', h, 1152 + 96 * h, 96))
for b in range(3):
    IN_BLOCKS.append(('sq', b, 1544 + 128 * b, 128))
for b in range(3):
    IN_BLOCKS.append(('sk', b, 1928 + 128 * b, 128))
for b in range(3):
    IN_BLOCKS.append(('sv', b, 2312 + 128 * b, 128))
for b in range(2):
    IN_BLOCKS.append(('u', b, 2696 + 128 * b, 128))
NB_IN = len(IN_BLOCKS)

PC_GMIX = 0
PC_GFFN = 8
PC_SBQG = 16
PC_SBKG = 17
PC_GLUB = 18
PC_S5G = 20
PC_S5D = 22
PC_FCW = 24
PC_GATEB = 90
PC_ARE = 98
PC_AIM = 106
PC_LDT = 114
PC_N = 122
Q_CW = 0
Q_CB = 32
Q_OG = 40
Q_N = 44


class KB:
    def __init__(self, nc, ctx, n_dma_sems=40):
        self.nc = nc
        self.ctx = ctx
        self.E = {'pe': nc.tensor, 'act': nc.scalar, 'dve': nc.vector, 'pool': nc.gpsimd, 'sp': nc.sync}
        self.sem = {}
        self.cnt = {}
        for e in self.E:
            self.sem[e] = ctx.enter_context(nc.semaphore("s_" + e))
            self.cnt[e] = 0
        self.dsem = [ctx.enter_context(nc.semaphore("d%d" % i)) for i in range(n_dma_sems)]
        self.dcnt = [0] * n_dma_sems
        self.dnext = 0
        self.seen = {e: {} for e in self.E}
        self.lastw = {}
        self.readers = {}
        self.nwaits = 0
        self.nins = 0
        self.inflight = {}

    def sb(self, name, shape, dt=F32):
        return self.ctx.enter_context(self.nc.sbuf_tensor(name, list(shape), dt))

    def ps(self, name, shape, dt=F32):
        return self.ctx.enter_context(self.nc.psum_tensor(name, list(shape), dt))

    @staticmethod
    def keys(aps):
        out = []
        for a in aps:
            if a is None or isinstance(a, (int, float)):
                continue
            if isinstance(a, (str, tuple)):
                out.append(a)
            else:
                out.append(a.name)
        return out

    def _wait(self, e, toks):
        best = {}
        for (sid, val) in toks:
            if sid == 'pe' and e == 'pe':
                continue
            if best.get(sid, 0) < val:
                best[sid] = val
        for sid, val in best.items():
            if self.seen[e].get(sid, 0) >= val:
                continue
            sem = self.sem[sid] if isinstance(sid, str) else self.dsem[sid]
            self.E[e].wait_ge(sem, val)
            self.seen[e][sid] = val
            self.nwaits += 1

    def _deps(self, r, w):
        toks = set()
        for k in r:
            if k in self.lastw:
                toks.add(self.lastw[k])
        for k in w:
            if k in self.lastw:
                toks.add(self.lastw[k])
            for t in self.readers.get(k, {}).items():
                toks.add(t)
        return toks

    def _record(self, tok, r, w):
        for k in r:
            d = self.readers.setdefault(k, {})
            if d.get(tok[0], 0) < tok[1]:
                d[tok[0]] = tok[1]
        for k in w:
            self.lastw[k] = tok
            self.readers[k] = {}

    def op(self, e, fn, *args, outs=(), ins=(), inc=True, **kw):
        r = self.keys(ins)
        w = self.keys(outs)
        self._wait(e, self._deps(r, w))
        ins_ = fn(*args, **kw)
        self.nins += 1
        if inc:
            self.cnt[e] += 1
            ins_.then_inc(self.sem[e], 1)
            tok = (e, self.cnt[e])
        else:
            tok = (e, self.cnt[e] + 1)
        self._record(tok, r, w)
        return ins_

    def dma(self, q, out, in_, ok=None, ik=None, **kw):
        j = self.dnext
        self.dnext = (self.dnext + 1) % len(self.dsem)
        r = self.keys([in_ if ik is None else ik])
        w = self.keys([out if ok is None else ok])
        toks = self._deps(r, w)
        if self.dcnt[j] > 0:
            toks.add((j, self.dcnt[j] * 16))
        fl = self.inflight.setdefault(q, [])
        lim = 4 if q == 'pool' else 12
        if len(fl) >= lim:
            toks.add(fl.pop(0))
        self._wait(q, toks)
        ins_ = self.E[q].dma_start(out=out, in_=in_, **kw)
        self.nins += 1
        self.dcnt[j] += 1
        ins_.then_inc(self.dsem[j], 16)
        tok = (j, self.dcnt[j] * 16)
        fl.append(tok)
        self._record(tok, r, w)
        return tok

    def barrier(self):
        toks = set()
        for e in self.E:
            if self.cnt[e] > 0:
                toks.add((e, self.cnt[e]))
        for j, c in enumerate(self.dcnt):
            if c > 0:
                toks.add((j, c * 16))
        for e in self.E:
            self._wait(e, toks)

    def wait_keys(self, e, aps):
        toks = set()
        for k in self.keys(aps):
            if k in self.lastw:
                toks.add(self.lastw[k])
        self._wait(e, toks)

    def mm(self, out, lhsT, rhs, start=True, stop=True, inc=True, **kw):
        return self.op('pe', self.nc.tensor.matmul, out, lhsT, rhs, start=start, stop=stop,
                       outs=[out], ins=[lhsT, rhs], inc=inc, **kw)

    def tr(self, out, in_, ident):
        return self.op('pe', self.nc.tensor.transpose, out, in_, ident, outs=[out], ins=[in_, ident])

    def act(self, out, in_, func, bias=None, scale=None, accum=None):
        kw = {}
        if bias is not None:
            kw['bias'] = bias
        if scale is not None:
            kw['scale'] = scale
        if accum is not None:
            kw['accum_out'] = accum
        return self.op('act', self.nc.scalar.activation, out, in_, func, outs=[out, accum],
                       ins=[in_, bias, scale], **kw)

    def tt(self, e, out, a, b, op):
        return self.op(e, self.E[e].tensor_tensor, out, a, b, op, outs=[out], ins=[a, b])

    def ts(self, e, out, a, s1, s2=None, op0=ALU.mult, op1=None):
        if op1 is None:
            return self.op(e, self.E[e].tensor_scalar, out, a, s1, None, op0, outs=[out], ins=[a, s1])
        return self.op(e, self.E[e].tensor_scalar, out, a, s1, s2, op0, op1, outs=[out], ins=[a, s1, s2])

    def stt(self, out, a, scalar, b, op0, op1):
        return self.op('dve', self.nc.vector.scalar_tensor_tensor, out, a, scalar, b, op0, op1,
                       outs=[out], ins=[a, scalar, b])

    def cp(self, e, out, in_):
        return self.op(e, self.E[e].tensor_copy, out, in_, outs=[out], ins=[in_])

    def ms(self, e, out, val):
        return self.op(e, self.E[e].memset, out, val, outs=[out], ins=[])

    def rcp(self, out, in_):
        return self.op('dve', self.nc.vector.reciprocal, out, in_, outs=[out], ins=[in_])

    def scan(self, out, d0, d1, init):
        return self.op('dve', self.nc.vector.tensor_tensor_scan, out, d0, d1, init, ALU.mult, ALU.add,
                       outs=[out], ins=[d0, d1, init])

    def asel(self, out, in_, pattern, cmp, fill, base, cm):
        return self.op('pool', self.nc.gpsimd.affine_select, out, in_, pattern=pattern, compare_op=cmp,
                       fill=fill, base=base, channel_multiplier=cm, outs=[out], ins=[in_])


def build(T=4096, L=2, enable=('ml', 'sb', 's5', 'ffn'), precast=True):
    import os
    precast = precast and not os.environ.get('NOPRECAST')
    DBG = int(os.environ.get('DBG', 99))
    NG = T // TG
    NT = T // 128
    nc = bass.Bass("TRN2", target_bir_lowering=False)
    dr = lambda name, shape, dt=F32, kind="ExternalInput": nc.dram_tensor(name, list(shape), dt, kind=kind).ap()
    x_d = dr("x", [T, D])
    win_d = dr("win", [L, NB_IN, 128, 1024])
    wout_d = dr("wout", [L, 12, 128, 1024])
    wup_d = dr("wup", [L, NB_UP, 128, 1024])
    wdn_d = dr("wdn", [L, NKD, 128, 1024])
    pp128_d = dr("pp128", [128, L, PC_N])
    pp96_d = dr("pp96", [96, L, Q_N])
    pp64_d = dr("pp64", [64, L, 6])
    s5pad_d = dr("s5pad", [L, 7, 128, 1024])
    gluw_d = dr("gluw", [L, 128, 2, 256])
    out_d = dr("out", [T, D], F32, "ExternalOutput")
    win_b = dr("win_b", [L, NB_IN, 128, 1024], BF16, "Internal")
    wout_b = dr("wout_b", [L, 12, 128, 1024], BF16, "Internal")
    wup_b = dr("wup_b", [L, NB_UP, 128, 1024], BF16, "Internal")
    wdn_b = dr("wdn_b", [L, NKD, 128, 1024], BF16, "Internal")

    DBGOUT = bool(os.environ.get('DBGOUT'))
    if DBGOUT:
        dbg_d = dr("dbg", [16, 128, 512], F32, "ExternalOutput")
    with ExitStack() as ctx:
        k = KB(nc, ctx)
        sb, ps = k.sb, k.ps

        def dbg(slot, ap, n):
            if DBGOUT:
                k.dma('sp', dbg_d[slot, 0:ap.shape[0], 0:n], ap, ok=('dbg', slot))
        ident = sb("ident", [128, 128])
        ones = sb("ones", [128, 128])
        onesbd = sb("onesbd", [128, 128])
        tri_le = sb("tri_le", [128, 128])
        negmask = sb("negmask", [128, 128])
        negU8 = sb("negU8", [128, 128], BF16)
        neg8 = sb("neg8", [128, 128], BF16)
        selneg = sb("selneg", [4, 4, 128])
        sbmask = sb("sbmask", [128, 4, 512], BF16)
        jrow = sb("jrow", [128, 128])
        k.ms('pool', ident[:], 0.0)
        k.asel(ident[:], ident[:], [[-1, 128]], ALU.not_equal, 1.0, 0, 1)
        k.ms('pool', ones[:], 1.0)
        k.ms('dve', onesbd[:], 0.0)
        k.ms('dve', onesbd[0:64, 0:64], 1.0)
        k.ms('dve', onesbd[64:128, 64:128], 1.0)
        k.ms('pool', tri_le[:], 1.0)
        k.asel(tri_le[:], tri_le[:], [[1, 128]], ALU.is_ge, 0.0, 0, -1)
        k.ms('pool', negmask[:], 0.0)
        k.asel(negmask[:], negmask[:], [[1, 128]], ALU.is_ge, -30000.0, 0, -1)
        k.ms('pool', negU8[:], -8.0)
        k.asel(negU8[:], negU8[:], [[-1, 128]], ALU.is_ge, 0.0, 0, 1)
        k.ms('pool', neg8[:], -8.0)
        k.ms('pool', selneg[:], 0.0)
        k.asel(selneg[:], selneg[:], [[-1, 4], [0, 128]], ALU.not_equal, -1.0, 0, 1)
        k.ms('pool', sbmask[:], 1.0)
        for j in range(4):
            k.asel(sbmask[:, j, :], sbmask[:, j, :], [[1, 512]], ALU.is_gt, 0.0, -128 * j, -1)
        jrow_i = sb("jrow_i", [128, 128], I32)
        k.op('pool', nc.gpsimd.iota, jrow_i[:], pattern=[[1, 128]], base=1, channel_multiplier=0,
             outs=[jrow_i], ins=[])
        k.cp('dve', jrow[:], jrow_i[:])

        pp128 = sb("pp128s", [128, L, PC_N])
        pp96 = sb("pp96s", [96, L, Q_N])
        pp64 = sb("pp64s", [64, L, 6])
        k.dma('sp', pp128[:], pp128_d)
        k.dma('sp', pp96[:], pp96_d)
        k.dma('sp', pp64[:], pp64_d)

        for l in range(L if precast else 0):
            for (src, dst, n) in ((win_d, win_b, NB_IN), (wout_d, wout_b, 12), (wup_d, wup_b, NB_UP), (wdn_d, wdn_b, NKD)):
                for b in range(n):
                    k.dma('pool', dst[l, b], src[l, b], ok=(dst.name, l, b))

        KT = sb("KT", [128, 3, T], BF16)
        Vr = sb("Vr", [128, NT, 384], BF16)
        Cst = [sb("Cst%d" % h, [96, 192]) for h in range(4)]
        s5st = sb("s5st", [128, 8, 2])
        halo_ml = sb("halo_ml", [96, 8, 3])
        halo_ff = sb("halo_ff", [128, NKD, 2])
        Bre = sb("Bre", [128, 8, 128]); Bim = sb("Bim", [128, 8, 128])
        Cre = sb("Cre", [128, 8, 128]); Cimn = sb("Cimn", [128, 8, 128])
        iti = sb("iti", [128, 256], I32)
        CS = sb("CS", [128, 8, 2, 128])
        s5r = sb("s5r", [128, 8])
        s5sm = [sb("s5sm%d" % i, [128, 8]) for i in range(6)]
        gluw = sb("gluws", [128, 2, 256])
        xt = [sb("xt%d" % i, [128, D]) for i in range(2)]
        xs = sb("xs", [128, D])
        ssum = sb("ssum", [128, 4]); rstd = sb("rstd", [128, 4])
        hT = sb("hT", [128, 8, TG], BF16)
        mix_ml = sb("mix_ml", [96, 4, TG], BF16)
        mix_sb = sb("mix_sb", [64, 6, TG], BF16)
        mix_s5 = sb("mix_s5", [128, 2, TG], BF16)
        NWP = 4
        wp = [sb("wp%d" % i, [128, 1024], BF16) for i in range(NWP)]
        wpi = [0]
        vtok = sb("vtok", [128, 4, 384])
        gps = sb("gps", [128, 4, 8])
        gi = sb("gi", [128, 4, 4]); spf = sb("spf", [128, 4, 4]); gtmp = sb("gtmp", [128, 4, 4])
        bcum = sb("bcum", [128, 4]); dcol = sb("dcol", [128, 4]); wcol = sb("wcol", [128, 4])
        bcumT = sb("bcumT", [4, 128])
        qT = sb("qT", [96, TG]); kT = sb("kT", [96, TG]); osig = sb("osig", [96, TG]); hml = sb("hml", [96, TG])
        et = [sb("et%d" % i, [128, 128]) for i in range(2)]
        pt = [sb("pt%d" % i, [128, 128]) for i in range(2)]
        ebq = [sb("ebq%d" % i, [96, 128]) for i in range(2)]
        qd = [sb("qd%d" % i, [96, 128]) for i in range(2)]
        dm = [sb("dm%d" % i, [96, 128]) for i in range(1)] * 2
        kw_ = [sb("kw%d" % i, [128, 96]) for i in range(2)]
        t5 = [sb("t5_%d" % i, [128, TG]) for i in range(6)]
        t5i = [0]
        uT = sb("uT", [128, 2, TG])
        spb = [uT[:, 0, :].bitcast(BF16)[:, 0:TG], uT[:, 0, :].bitcast(BF16)[:, TG:2 * TG]]
        spaccb = [uT[:, 1, :].bitcast(BF16)[:, 0:TG], uT[:, 1, :].bitcast(BF16)[:, TG:2 * TG]]
        sq_bf = sb("sq_bf", [128, 3, TG], BF16)
        sp_sb = [sb("sp_sb%d" % i, [128, TG]) for i in range(2)]
        spacc = [sb("spacc%d" % i, [128, TG]) for i in range(2)]
        at_sb = [sb("at_sb%d" % i, [128, TG], BF16) for i in range(2)]
        aT = sb("aT", [128, NKD, TG], BF16)
        gst = [sb("gst%d" % i, [128, TG + 3]) for i in range(2)]
        stg = [g_[0:96, :] for g_ in gst]
        pb = [ps("pb%d" % i, [128, 512]) for i in range(8)]
        rot = [0]

        def nps(lst=(0, 1, 2, 3, 4)):
            rot[0] = (rot[0] + 1) % len(lst)
            return pb[lst[rot[0]]]

        def ntmp():
            t5i[0] = (t5i[0] + 1) % len(t5)
            return t5[t5i[0]]

        f32src = {win_b.name: win_d, wout_b.name: wout_d, wup_b.name: wup_d, wdn_b.name: wdn_d}

        def load_w(src, src_key=None):
            b = wp[wpi[0]]
            wpi[0] = (wpi[0] + 1) % NWP
            if precast:
                k.dma('sp', b[:], src, ik=src_key)
            else:
                k.dma('pool', b[:], f32src[src_key[0]][src_key[1], src_key[2]])
            return b

        def norm_T(l, g, src_d, gcol, src_name):
            t0 = g * TG
            for j in range(4):
                xb = xt[j % 2]
                k.dma('sp', xb[:], src_d[t0 + 128 * j: t0 + 128 * (j + 1), :], ik=(src_name, 4 * g + j))
                k.act(xs[:], xb[:], AF.Square, accum=ssum[:, j:j + 1])
                k.ts('dve', rstd[:, j:j + 1], ssum[:, j:j + 1], 1.0 / D, EPS, ALU.mult, ALU.add)
                k.act(rstd[:, j:j + 1], rstd[:, j:j + 1], AF.Sqrt)
                k.rcp(rstd[:, j:j + 1], rstd[:, j:j + 1])
                k.ts('dve', xs[:], xb[:], rstd[:, j:j + 1], None, ALU.mult)
                for half in range(2):
                    p = nps()
                    for c in range(4):
                        kc = half * 4 + c
                        k.tr(p[:, 128 * c:128 * (c + 1)], xs[:, 128 * kc:128 * (kc + 1)], ident[:])
                    gb = pp128[:, l, gcol + 4 * half: gcol + 4 * half + 4].unsqueeze(2).to_broadcast([128, 4, 128])
                    k.tt('dve', hT[:, 4 * half:4 * half + 4, 128 * j:128 * (j + 1)],
                         p[:].rearrange("p (c t) -> p c t", c=4), gb, ALU.mult)

        def proj_fm(wb, M, p):
            wv = wb[:].rearrange("p (k c) -> p k c", k=8)
            for kc in range(8):
                k.mm(p[0:M, :], wv[:, kc, 0:M], hT[:, kc, :], start=(kc == 0), stop=(kc == 7), inc=(kc == 7))

        def proj_tm(wb, ncols, p):
            wv = wb[:].rearrange("p (k c) -> p k c", k=8)
            pv = p[:].rearrange("p (j c) -> p j c", j=4)
            for j in range(4):
                for kc in range(8):
                    k.mm(pv[:, j, 0:ncols], hT[:, kc, 128 * j:128 * (j + 1)], wv[:, kc, 0:ncols],
                         start=(kc == 0), stop=(kc == 7), inc=(kc == 7))
            return pv

        def rms_fm(src_sb, sq_src, n, onesm, out_ap, gscalar, extra=None):
            sq = ntmp()
            k.act(sq[0:n, :], sq_src, AF.Square)
            p = nps()
            k.mm(p[0:n, :], onesm, sq[0:n, :])
            r = ntmp()
            k.ts('dve', r[0:n, :], p[0:n, :], 1.0 / (n if n != 128 else 64), EPS, ALU.mult, ALU.add)
            k.act(r[0:n, :], r[0:n, :], AF.Sqrt)
            k.rcp(r[0:n, :], r[0:n, :])
            if extra is None:
                k.stt(out_ap, src_sb, gscalar, r[0:n, :], ALU.mult, ALU.mult)
            else:
                k.tt('dve', r[0:n, :], r[0:n, :], extra, ALU.mult)
                k.stt(out_ap, src_sb, gscalar, r[0:n, :], ALU.mult, ALU.mult)

        def lam_bar(are, aim, ldt, F, o_re, o_im, o_cr, o_ci, tmp):
            t0_, t1_, t2_, ti = tmp
            k.ts('dve', are, are, -1e-4, None, ALU.min)
            k.act(ldt, ldt, AF.Exp)
            k.tt('dve', t0_, are, ldt, ALU.mult)
            k.act(t0_, t0_, AF.Exp)
            k.tt('dve', t1_, aim, ldt, ALU.mult)

            def sin_of(dst, src, shift):
                k.ts('dve', t2_, src, 1.0 / TWO_PI, shift / TWO_PI, ALU.mult, ALU.add)
                k.cp('dve', ti, t2_)
                k.cp('dve', t2_, ti)
                k.ts('dve', t2_, t2_, -TWO_PI, shift, ALU.mult, ALU.add)
                k.tt('dve', t2_, t2_, src, ALU.add)
                k.act(dst, t2_, AF.Sin)
            sin_of(o_im, t1_, 0.0)
            sin_of(o_re, t1_, math.pi / 2)
            k.tt('dve', o_re, o_re, t0_, ALU.mult)
            k.tt('dve', o_im, o_im, t0_, ALU.mult)
            if o_cr is None:
                return
            k.tt('dve', t0_, are, are, ALU.mult)
            k.tt('dve', t1_, aim, aim, ALU.mult)
            k.tt('dve', t0_, t0_, t1_, ALU.add)
            k.rcp(t0_, t0_)
            k.ts('dve', t1_, o_re, -1.0, None, ALU.add)
            k.tt('dve', o_cr, t1_, are, ALU.mult)
            k.tt('dve', t2_, o_im, aim, ALU.mult)
            k.tt('dve', o_cr, o_cr, t2_, ALU.add)
            k.tt('dve', o_cr, o_cr, t0_, ALU.mult)
            k.tt('dve', o_ci, o_im, are, ALU.mult)
            k.tt('dve', t2_, t1_, aim, ALU.mult)
            k.tt('dve', o_ci, o_ci, t2_, ALU.subtract)
            k.tt('dve', o_ci, o_ci, t0_, ALU.mult)

        def s5_setup(l):
            f = lambda t: t[:].rearrange("p a b -> p (a b)")
            k.dma('sp', f(Cre), s5pad_d[l, 5]); k.dma('sp', f(Cimn), s5pad_d[l, 6])
            k.dma('sp', gluw[:], gluw_d[l])
            k.ts('dve', f(Cimn), f(Cimn), -1.0, None, ALU.mult)
            for hf in range(4):
                cs = slice(256 * hf, 256 * (hf + 1))
                H = slice(0, 256)
                a0, a1, a2, lbr, lbi, T0, T1, T2 = t5[0][:, H], t5[1][:, H], t5[2][:, H], t5[3][:, H], t5[4][:, H], t5[5][:, H], sp_sb[0][:, H], sp_sb[1][:, H]
                cr, ci = spacc[0][:, H], spacc[1][:, H]
                k.dma('sp', a0, s5pad_d[l, 0][:, cs]); k.dma('sp', a1, s5pad_d[l, 1][:, cs])
                k.dma('sp', a2, s5pad_d[l, 2][:, cs])
                k.dma('sp', f(Bre)[:, cs], s5pad_d[l, 3][:, cs]); k.dma('sp', f(Bim)[:, cs], s5pad_d[l, 4][:, cs])
                lam_bar(a0, a1, a2, 256, lbr, lbi, cr, ci, [T0, T1, T2, iti[:]])
                k.tt('dve', T0, cr, f(Bre)[:, cs], ALU.mult)
                k.tt('dve', T1, ci, f(Bim)[:, cs], ALU.mult)
                k.tt('dve', T0, T0, T1, ALU.subtract)
                k.tt('dve', T1, cr, f(Bim)[:, cs], ALU.mult)
                k.tt('dve', T2, ci, f(Bre)[:, cs], ALU.mult)
                k.tt('dve', f(Bim)[:, cs], T1, T2, ALU.add)
                k.cp('dve', f(Bre)[:, cs], T0)
            are = s5sm[0]; aim = s5sm[1]; ldt = s5sm[2]
            k.cp('dve', are[:], pp128[:, l, PC_ARE:PC_ARE + 8])
            k.cp('dve', aim[:], pp128[:, l, PC_AIM:PC_AIM + 8])
            k.cp('dve', ldt[:], pp128[:, l, PC_LDT:PC_LDT + 8])
            k.ts('dve', are[:], are[:], -1e-4, None, ALU.min)
            k.act(ldt[:], ldt[:], AF.Exp)
            k.tt('dve', s5r[:], are[:], ldt[:], ALU.mult)
            k.act(s5r[:], s5r[:], AF.Exp)
            th = s5sm[3]
            k.tt('dve', th[:], aim[:], ldt[:], ALU.mult)
            fh = s5sm[4]; fl = s5sm[5]
            k.ts('dve', fh[:], th[:], 2048.0 / TWO_PI, None, ALU.mult)
            k.cp('dve', iti[:, 0:8], fh[:])
            k.cp('dve', fh[:], iti[:, 0:8])
            k.ts('dve', fh[:], fh[:], 1.0 / 2048.0, None, ALU.mult)
            k.ts('dve', fl[:], th[:], 1.0 / TWO_PI, None, ALU.mult)
            k.tt('dve', fl[:], fl[:], fh[:], ALU.subtract)
            tA = t5[0][:, 0:128]; tB = t5[1][:, 0:128]; tI = iti[:, 0:128]
            for i in range(8):
                for (dsti, shift) in ((1, 0.0), (0, 0.25)):
                    k.ts('dve', tA, jrow[:], fh[:, i:i + 1], None, ALU.mult)
                    k.cp('dve', tI, tA)
                    k.cp('dve', tB, tI)
                    k.tt('dve', tA, tA, tB, ALU.subtract)
                    k.stt(tA, jrow[:], fl[:, i:i + 1], tA, ALU.mult, ALU.add)
                    if shift != 0.0:
                        k.ts('dve', tA, tA, shift, None, ALU.add)
                    k.cp('dve', tI, tA)
                    k.cp('dve', tB, tI)
                    k.tt('dve', tA, tA, tB, ALU.subtract)
                    k.act(CS[:, i, dsti, :], tA, AF.Sin, scale=TWO_PI)
            if l == 0:
                dbg(0, Bre[:, 0, :], 128); dbg(1, Bim[:, 0, :], 128); dbg(2, Bre[:, 5, :], 128)
                pass
                dbg(7, s5r[:], 8)

        def mlstm_head(l, g, h):
            def SA(j):
                cs = slice(128 * j, 128 * (j + 1))
                b = j % 2
                pB = nps()
                k.mm(pB[:, 0:128], selneg[:, h, :], bcumT_all[j][:, :], start=True, stop=False)
                k.mm(pB[:, 0:128], ident[:], negmask[:], start=False, stop=True)
                k.act(et[b][:], pB[:, 0:128], AF.Exp, bias=dcol_all[:, j, h:h + 1])
                k.mm(pB[0:96, 128:256], selneg[:, h, 0:96], bcumT_all[j][:, :])
                k.act(ebq[b][:], pB[0:96, 128:256], AF.Exp)
                k.tt('dve', qd[b][:], qT[:, cs], ebq[b][:], ALU.mult)
                pS = nps()
                k.mm(pS[:, 0:128], kT[:, cs], qT[:, cs])
                k.tt('dve', pt[b][:], pS[:, 0:128], et[b][:], ALU.mult)
                k.tr(pS[:, 128:224], kT[:, cs], ident[0:96, 0:96])
                k.ts('dve', kw_[b][:], pS[:, 128:224], wcol_all[:, j, h:h + 1], None, ALU.mult)

            def SB(j):
                cs = slice(128 * j, 128 * (j + 1))
                b = j % 2
                pN = nps()
                k.mm(pN[0:96, 0:128], vtok[:, j, 96 * h:96 * (h + 1)], pt[b][:], start=True, stop=False)
                k.mm(pN[0:96, 0:128], Cst[h][:, 0:96], qd[b][:], start=False, stop=True)
                k.mm(pN[0:96, 128:256], ones[:, 0:96], pt[b][:], start=True, stop=False)
                k.mm(pN[0:96, 128:256], Cst[h][:, 96:192], qd[b][:], start=False, stop=True)
                k.mm(pN[0:96, 256:352], kw_[b][:], vtok[:, j, 96 * h:96 * (h + 1)])
                k.mm(pN[0:96, 352:448], kw_[b][:], ones[:, 0:96])
                k.stt(Cst[h][:], Cst[h][:], ebq[b][:, 127:128], pN[0:96, 256:448], ALU.mult, ALU.add)
                k.ts('dve', dm[b][:], pN[0:96, 128:256], 1.0, None, ALU.max)
                k.stt(dm[b][:], pN[0:96, 128:256], -1.0, dm[b][:], ALU.mult, ALU.max)
                k.rcp(dm[b][:], dm[b][:])
                k.tt('dve', hml[:, cs], pN[0:96, 0:128], dm[b][:], ALU.mult)

            SA(0)
            for j in range(4):
                if j + 1 < 4:
                    SA(j + 1)
                SB(j)
            rms_fm(hml[:], hml[:], 96, ones[0:96, 0:96], mix_ml[:, h, :], pp96[:, l, Q_OG + h:Q_OG + h + 1], extra=osig[:])

        def gates_prep(l):
            gb = pp128[:, l, PC_GATEB:PC_GATEB + 8]
            k.tt('dve', gi[:], gps[:, :, 0:4], gb[:, 0:4].unsqueeze(1).to_broadcast([128, 4, 4]), ALU.add)
            k.ts('dve', gi[:], gi[:], -0.5 * math.log(96.0), None, ALU.add)
            k.tt('dve', gtmp[:], gps[:, :, 4:8], gb[:, 4:8].unsqueeze(1).to_broadcast([128, 4, 4]), ALU.add)
            k.act(gtmp[:], gtmp[:], AF.Exp, scale=-1.0)
            k.act(spf[:], gtmp[:], AF.Ln, bias=1.0)
            for j in range(4):
                p = nps()
                k.mm(p[:, 0:4], tri_le[:], spf[:, j, :])
                k.mm(p[:, 4:8], ones[:], spf[:, j, :])
                k.mm(p[0:4, 128:256], spf[:, j, :], tri_le[:])
                k.tt('dve', dcol_all[:, j, :], gi[:, j, :], p[:, 0:4], ALU.add)
                k.tt('dve', wcol_all[:, j, :], dcol_all[:, j, :], p[:, 4:8], ALU.subtract)
                k.act(wcol_all[:, j, :], wcol_all[:, j, :], AF.Exp)
                k.cp('dve', bcumT_all[j][:, :], p[0:4, 128:256])

        dcol_all = sb("dcol_all", [128, 4, 4]); wcol_all = sb("wcol_all", [128, 4, 4])
        bcumT_all = [sb("bcumT%d" % j, [4, 128]) for j in range(4)]

        def sb_attention(l, g):
            for h in range(6):
                pr = h // 2
                lo = 64 * (h % 2)
                qh = sq_bf[lo:lo + 64, pr, :]
                O = pb[7]
                kbs = list(range(4 * g + 3, -1, -1))
                n = len(kbs)

                def kt_of(idx):
                    kb = kbs[idx]
                    return KT[lo:lo + 64, pr, 128 * kb:128 * (kb + 1)]

                def S1(idx):
                    b = idx % 2
                    kb = kbs[idx]
                    Z = pb[idx % 4]
                    k.mm(Z[:], kt_of(idx), qh, start=True, stop=False)
                    k.act(sp_sb[b][:], Z[:], AF.Exp, scale=0.125)
                    k.act(spb[b], sp_sb[b][:], AF.Ln, bias=1.0)
                    if kb >= 4 * g:
                        k.tt('dve', spb[b], spb[b], sbmask[:, kb - 4 * g, :], ALU.mult)

                def S2(idx):
                    b = idx % 2
                    kb = kbs[idx]
                    A = pb[idx % 4]
                    k.mm(A[:], negU8[:], spb[b], start=False, stop=(idx == 0))
                    if idx > 0:
                        k.mm(A[:], neg8[:], spaccb[1 - b], start=False, stop=True)
                        if idx + 1 < n:
                            k.tt('dve', spacc[b][:], spacc[1 - b][:], spb[b], ALU.add)
                            k.cp('dve', spaccb[b], spacc[b][:])
                    else:
                        k.cp('dve', spacc[b][:], spb[b])
                        k.cp('dve', spaccb[b], spb[b])
                    k.act(at_sb[b][:], A[:], AF.Exp, scale=0.125)
                    if kb >= 4 * g:
                        k.tt('dve', at_sb[b][:], at_sb[b][:], sbmask[:, kb - 4 * g, :], ALU.mult)

                def S3(idx):
                    b = idx % 2
                    kb = kbs[idx]
                    k.mm(O[0:64, :], Vr[:, kb, 64 * h:64 * (h + 1)], at_sb[b][:], start=(idx == 0), stop=(idx == n - 1))

                S1(0)
                for idx in range(n):
                    if idx + 1 < n:
                        S1(idx + 1)
                    S2(idx)
                    S3(idx)
                osb = ntmp()
                k.act(osb[0:64, :], O[0:64, :], AF.Copy)
                rms_fm(osb[0:64, :], O[0:64, :], 64, ones[0:64, 0:64], mix_sb[:, h, :], pp64[:, l, h:h + 1])

        def s5_group(l, g):
            Y = [pb[5], pb[6]]
            W = 128
            T2 = hT[:].rearrange("p a b -> p (a b)").bitcast(F32).rearrange("p (i c w) -> p i c w", i=8, c=2)
            k.cp('pool', T2[:, :, 0, :], CS[:, :, 1, :])
            k.ts('pool', T2[:, :, 1, :], CS[:, :, 0, :], -1.0, None, ALU.mult)
            its = [(sc, i) for sc in range(4) for i in range(8)]
            banks = {}
            bufs = {}

            def P(n):
                sc, i = its[n]
                c = i // 4
                hs = slice(W * sc, W * (sc + 1))
                pR = nps()
                banks[n] = pR
                k.mm(pR[:, 0:W], Bre[:, i, :], uT[:, c, hs])
                k.mm(pR[:, W:2 * W], Bim[:, i, :], uT[:, c, hs])

            def V(n):
                sc, i = its[n]
                pR = banks[n]
                bA = ntmp(); bB = ntmp(); bC = ntmp()
                v3 = lambda ap: ap.rearrange("p (c w) -> p c w", c=2)
                bc = lambda ap: ap.unsqueeze(1).to_broadcast([128, 2, W])
                t1 = bA[:, 0:2 * W]; t2 = bA[:, 2 * W:4 * W]
                m = bB[:, 0:2 * W]; sr_ = bB[:, 2 * W:3 * W]; sm_ = bB[:, 3 * W:4 * W]
                u1 = bC[:, 0:2 * W]; u2 = bC[:, 2 * W:4 * W]
                so = bA[:, 0:2 * W]
                k.tt('dve', v3(t1), bc(pR[:, 0:W]), CS[:, i], ALU.mult)
                k.tt('dve', v3(t2), bc(pR[:, W:2 * W]), T2[:, i], ALU.mult)
                k.tt('dve', m, t1, t2, ALU.add)
                rb = s5r[:, i:i + 1].to_broadcast([128, W])
                k.scan(sr_, rb, m[:, 0:W], s5st[:, i, 0:1])
                k.scan(sm_, rb, m[:, W:2 * W], s5st[:, i, 1:2])
                k.tt('pool', v3(u1), bc(sr_), CS[:, i], ALU.mult)
                k.tt('pool', v3(u2), bc(sm_), T2[:, i], ALU.mult)
                k.tt('pool', so, u1, u2, ALU.add)
                k.cp('pool', s5st[:, i, 0:1], so[:, W - 1:W])
                k.ts('pool', s5st[:, i, 1:2], so[:, 2 * W - 1:2 * W], -1.0, None, ALU.mult)
                bufs[n] = so

            def C(n):
                sc, i = its[n]
                c = i // 4
                hs = slice(W * sc, W * (sc + 1))
                so = bufs[n]
                k.mm(Y[c][:, hs], Cre[:, i, :], so[:, 0:W], start=(i % 4 == 0), stop=False)
                k.mm(Y[c][:, hs], Cimn[:, i, :], so[:, W:2 * W], start=False, stop=(i % 4 == 3))

            P(0)
            for n in range(len(its)):
                if n + 1 < len(its):
                    P(n + 1)
                V(n)
                C(n)
            if l == 0 and g == 0:
                yd = ntmp()
                k.cp('dve', yd[:], Y[0][:]); dbg(8, yd[:], 512)
                yd2 = ntmp()
                k.cp('dve', yd2[:], Y[1][:]); dbg(14, yd2[:], 512)
                dbg(9, uT[:, 0, :], 512); dbg(13, uT[:, 1, :], 512)
            yv = [sp_sb[0], sp_sb[1]]
            for c in range(2):
                y = yv[c]
                k.stt(y[:], uT[:, c, :], pp128[:, l, PC_S5D + c:PC_S5D + c + 1], Y[c][:], ALU.mult, ALU.add)
                a = ntmp()
                k.tt('dve', a[:], y[:], y[:], ALU.mult)
                k.ts('dve', a[:], a[:], 0.044715, 1.0, ALU.mult, ALU.add)
                k.tt('dve', a[:], a[:], y[:], ALU.mult)
                k.act(a[:], a[:], AF.Sigmoid, scale=2.0 * math.sqrt(2.0 / math.pi))
                k.tt('dve', y[:], y[:], a[:], ALU.mult)
                if l == 0 and g == 0 and c == 0:
                    dbg(10, y[:], 512)
            sqs = []
            for c in range(2):
                p = nps()
                k.mm(p[:], gluw[:, 0, 128 * c:128 * (c + 1)], yv[0][:], start=True, stop=False)
                k.mm(p[:], gluw[:, 1, 128 * c:128 * (c + 1)], yv[1][:], start=False, stop=True)
                s = ntmp()
                k.act(s[:], p[:], AF.Sigmoid, bias=pp128[:, l, PC_GLUB + c:PC_GLUB + c + 1])
                sqs.append(s)
            for c in range(2):
                s = sqs[c]
                k.tt('dve', yv[c][:], yv[c][:], s[:], ALU.mult)
                if l == 0 and g == 0 and c == 0:
                    dbg(11, yv[c][:], 512)
                k.act(s[:], yv[c][:], AF.Square)
            p = nps()
            k.mm(p[:], ones[:], sqs[0][:], start=True, stop=False)
            k.mm(p[:], ones[:], sqs[1][:], start=False, stop=True)
            r = spacc[0]
            k.ts('dve', r[:], p[:], 1.0 / 256, EPS, ALU.mult, ALU.add)
            k.act(r[:], r[:], AF.Sqrt)
            k.rcp(r[:], r[:])
            if l == 0 and g == 0:
                dbg(12, r[:], 512)
            for c in range(2):
                k.stt(mix_s5[:, c, :], yv[c][:], pp128[:, l, PC_S5G + c:PC_S5G + c + 1], r[:], ALU.mult, ALU.mult)

        for h in range(4):
            k.ms('dve', Cst[h][:], 0.0)
        for l in range(L):
            src_d = x_d if l == 0 else out_d
            src_name = 'x' if l == 0 else 'out'
            if 's5' in enable:
                s5_setup(l)
            k.ms('dve', s5st[:], 0.0)
            k.ms('dve', halo_ml[:], 0.0)
            k.ms('dve', halo_ff[:], 0.0)
            for h in range(4):
                k.ms('dve', Cst[h][:], 0.0)
            for g in range(NG):
                t0 = g * TG
                norm_T(l, g, src_d, PC_GMIX, src_name)
                if 'ml' not in enable:
                    k.ms('pool', mix_ml[:], 0.0)
                if 'sb' not in enable:
                    k.ms('pool', mix_sb[:], 0.0)
                if 's5' not in enable:
                    k.ms('pool', mix_s5[:], 0.0)
                for bi_, (nm, idx, col0, ncols) in enumerate(IN_BLOCKS):
                    grp = {'v': 'ml', 'if': 'ml', 'q': 'ml', 'k': 'ml', 'o': 'ml', 'sq': 'sb', 'sk': 'sb', 'sv': 'sb', 'u': 's5'}[nm]
                    if grp not in enable:
                        continue
                    wb = load_w(win_b[l, bi_], (win_b.name, l, bi_))
                    p = nps()
                    if nm == 'v':
                        pv = proj_tm(wb, 128, p)
                        k.cp('dve', vtok[:, :, 128 * idx:128 * (idx + 1)], pv[:, :, :])
                    elif nm == 'if':
                        pv = proj_tm(wb, 8, p)
                        k.cp('dve', gps[:], pv[:, :, 0:8])
                        if DBG >= 1:
                            gates_prep(l)
                    elif nm in ('q', 'k'):
                        if DBG < 2:
                            continue
                        proj_fm(wb, 96, p)
                        blk = idx if nm == 'q' else 4 + idx
                        s_ = stg[bi_ % 2]
                        cv = ntmp()[0:96, :]
                        k.act(s_[:, 3:TG + 3], p[0:96, :], AF.Copy)
                        k.cp('dve', s_[:, 0:3], halo_ml[:, blk, :])
                        k.cp('dve', halo_ml[:, blk, :], s_[:, TG:TG + 3])
                        cw = pp96[:, l, Q_CW + 4 * blk:Q_CW + 4 * blk + 4]
                        k.ts('dve', cv, s_[:, 3:TG + 3], cw[:, 3:4], pp96[:, l, Q_CB + blk:Q_CB + blk + 1], ALU.mult, ALU.add)
                        for kk in range(3):
                            k.stt(cv, s_[:, kk:kk + TG], cw[:, kk:kk + 1], cv, ALU.mult, ALU.add)
                        k.act((qT if nm == 'q' else kT)[:], cv, AF.Silu)
                    elif nm == 'o':
                        if DBG < 3:
                            continue
                        proj_fm(wb, 96, p)
                        k.act(osig[:], p[0:96, :], AF.Sigmoid)
                        if DBG >= 4:
                            mlstm_head(l, g, idx)
                    elif nm in ('sq', 'sk'):
                        proj_fm(wb, 128, p)
                        raw = ntmp()
                        k.act(raw[:], p[:], AF.Copy)
                        gcol = PC_SBQG if nm == 'sq' else PC_SBKG
                        dst = sq_bf[:, idx, :] if nm == 'sq' else KT[:, idx, t0:t0 + TG]
                        rms_fm(raw[:], p[:], 128, onesbd[:], dst, pp128[:, l, gcol:gcol + 1])
                    elif nm == 'sv':
                        pv = proj_tm(wb, 128, p)
                        k.cp('dve', Vr[:, 4 * g:4 * g + 4, 128 * idx:128 * (idx + 1)], pv[:, :, :])
                        if idx == 2 and DBG >= 11:
                            sb_attention(l, g)
                    elif nm == 'u':
                        proj_fm(wb, 128, p)
                        k.act(uT[:, idx, :], p[:], AF.Copy)
                        if idx == 1:
                            s5_group(l, g)
                for b in range(12):
                    wb = load_w(wout_b[l, b], (wout_b.name, l, b))
                    for j in range(4):
                        cs = slice(128 * j, 128 * (j + 1))
                        if b < 4:
                            lhs = mix_ml[:, b, cs]; rows = 96
                        elif b < 10:
                            lhs = mix_sb[:, b - 4, cs]; rows = 64
                        else:
                            lhs = mix_s5[:, b - 10, cs]; rows = 128
                        for half in range(2):
                            k.mm(pb[2 * j + half][:], lhs, wb[0:rows, 512 * half:512 * (half + 1)],
                                 start=(b == 0), stop=(b == 11))
                for j in range(4):
                    xb = xt[j % 2]
                    k.dma('sp', xb[:], src_d[t0 + 128 * j: t0 + 128 * (j + 1), :], ik=(src_name, 4 * g + j))
                    for half in range(2):
                        k.tt('dve', xb[:, 512 * half:512 * (half + 1)], xb[:, 512 * half:512 * (half + 1)],
                             pb[2 * j + half][:], ALU.add)
                    k.dma('sp', out_d[t0 + 128 * j: t0 + 128 * (j + 1), :], xb[:], ok=('out', 4 * g + j))
                if 'ffn' not in enable:
                    continue
                norm_T(l, g, out_d, PC_GFFN, 'out')
                for b in range(NKD):
                    wg = load_w(wup_b[l, 2 * b], (wup_b.name, l, 2 * b))
                    wv_ = load_w(wup_b[l, 2 * b + 1], (wup_b.name, l, 2 * b + 1))
                    pg = nps(); pv_ = nps()
                    proj_fm(wg, 128, pg)
                    proj_fm(wv_, 128, pv_)
                    s_ = gst[b % 2]
                    k.act(s_[:, 2:TG + 2], pg[:], AF.Copy)
                    k.cp('dve', s_[:, 0:2], halo_ff[:, b, :])
                    k.cp('dve', halo_ff[:, b, :], s_[:, TG:TG + 2])
                    cw = pp128[:, l, PC_FCW + 3 * b:PC_FCW + 3 * b + 3]
                    c_ = ntmp()
                    k.ts('dve', c_[:], s_[:, 2:TG + 2], cw[:, 2:3], None, ALU.mult)
                    for kk in range(2):
                        k.stt(c_[:], s_[:, kk:kk + TG], cw[:, kk:kk + 1], c_[:], ALU.mult, ALU.add)
                    k.act(c_[:], c_[:], AF.Silu)
                    k.tt('dve', aT[:, b, :], c_[:], pv_[:], ALU.mult)
                for kc in range(NKD):
                    wd = load_w(wdn_b[l, kc], (wdn_b.name, l, kc))
                    for j in range(4):
                        for half in range(2):
                            k.mm(pb[2 * j + half][:], aT[:, kc, 128 * j:128 * (j + 1)], wd[:, 512 * half:512 * (half + 1)],
                                 start=(kc == 0), stop=(kc == NKD - 1), inc=(kc == NKD - 1 or (j == 3 and half == 1)))
                for j in range(4):
                    xb = xt[j % 2]
                    k.dma('sp', xb[:], out_d[t0 + 128 * j: t0 + 128 * (j + 1), :], ik=('out', 4 * g + j))
                    for half in range(2):
                        k.tt('dve', xb[:, 512 * half:512 * (half + 1)], xb[:, 512 * half:512 * (half + 1)],
                             pb[2 * j + half][:], ALU.add)
                    k.dma('sp', out_d[t0 + 128 * j: t0 + 128 * (j + 1), :], xb[:], ok=('out', 4 * g + j))
        k.barrier()
        build.stats = (k.nins, k.nwaits)
    return nc


def prep_shared(inp, L=2):
    f = np.float32
    w_in = np.asarray(inp['w_in'], f)
    win = np.zeros((L, NB_IN, 128, 8, 128), f)
    for bi_, (nm, idx, col0, ncols) in enumerate(IN_BLOCKS):
        win[:, bi_, :, :, :ncols] = w_in[:, :, col0:col0 + ncols].reshape(L, 8, 128, ncols).transpose(0, 2, 1, 3)
    w_out = np.asarray(inp['w_out'], f)
    wout = np.zeros((L, 12, 128, 1024), f)
    for h in range(4):
        wout[:, h, :96] = w_out[:, 96 * h:96 * (h + 1)]
    for h in range(6):
        wout[:, 4 + h, :64] = w_out[:, 384 + 64 * h:384 + 64 * (h + 1)]
    for c in range(2):
        wout[:, 10 + c] = w_out[:, 768 + 128 * c:768 + 128 * (c + 1)]
    w_up = np.asarray(inp['ffn_w_up'], f)
    wup = np.zeros((L, NB_UP, 128, 8, 128), f)
    for b in range(NKD):
        wup[:, 2 * b] = w_up[:, :, 128 * b:128 * (b + 1)].reshape(L, 8, 128, 128).transpose(0, 2, 1, 3)
        wup[:, 2 * b + 1] = w_up[:, :, DFF + 128 * b:DFF + 128 * (b + 1)].reshape(L, 8, 128, 128).transpose(0, 2, 1, 3)
    wdn = np.ascontiguousarray(np.asarray(inp['ffn_w_down'], f).reshape(L, NKD, 128, 1024))
    pp128 = np.zeros((128, L, PC_N), f)
    pp96 = np.zeros((96, L, Q_N), f)
    pp64 = np.zeros((64, L, 6), f)
    s5pad = np.zeros((L, 7, 128, 8, 128), f)
    for l in range(L):
        pp128[:, l, PC_GMIX:PC_GMIX + 8] = np.asarray(inp['norm_mix_g'], f)[l].reshape(8, 128).T
        pp128[:, l, PC_GFFN:PC_GFFN + 8] = np.asarray(inp['norm_ffn_g'], f)[l].reshape(8, 128).T
        pp128[:, l, PC_SBQG] = np.tile(np.asarray(inp['sb_q_g'], f)[l], 2)
        pp128[:, l, PC_SBKG] = np.tile(np.asarray(inp['sb_k_g'], f)[l], 2)
        pp128[:, l, PC_GLUB:PC_GLUB + 2] = np.asarray(inp['s5_glu_b'], f)[l].reshape(2, 128).T
        pp128[:, l, PC_S5G:PC_S5G + 2] = np.asarray(inp['s5_out_g'], f)[l].reshape(2, 128).T
        pp128[:, l, PC_S5D:PC_S5D + 2] = np.asarray(inp['s5_d'], f)[l].reshape(2, 128).T
        fcw = np.asarray(inp['ffn_conv_w'], f)[l]
        pp128[:, l, PC_FCW:PC_FCW + 66] = fcw.reshape(3, NKD, 128).transpose(2, 1, 0).reshape(128, 66)
        pp128[:, l, PC_GATEB:PC_GATEB + 8] = np.asarray(inp['ml_gate_b'], f)[l][None, :]
        are = np.asarray(inp['s5_a_re'], f)[l]; aim = np.asarray(inp['s5_a_im'], f)[l]
        ldt = np.repeat(np.asarray(inp['s5_log_dt'], f)[l][:, None], 64, axis=1)
        pp128[:, l, PC_ARE:PC_ARE + 8] = are.reshape(8, 128).T
        pp128[:, l, PC_AIM:PC_AIM + 8] = aim.reshape(8, 128).T
        pp128[:, l, PC_LDT:PC_LDT + 8] = ldt.reshape(8, 128).T
        cw = np.asarray(inp['ml_conv_w'], f)[l]
        pp96[:, l, Q_CW:Q_CW + 32] = cw.reshape(4, 8, 96).transpose(2, 1, 0).reshape(96, 32)
        pp96[:, l, Q_CB:Q_CB + 8] = np.asarray(inp['ml_conv_b'], f)[l].reshape(8, 96).T
        pp96[:, l, Q_OG:Q_OG + 4] = np.asarray(inp['ml_out_g'], f)[l].T
        pp64[:, l, :] = np.asarray(inp['sb_out_g'], f)[l].T
        s5pad[l, 0] = are.reshape(1, 8, 128)
        s5pad[l, 1] = aim.reshape(1, 8, 128)
        s5pad[l, 2] = ldt.reshape(1, 8, 128)
        bre = np.asarray(inp['s5_b_re'], f)[l]; bim = np.asarray(inp['s5_b_im'], f)[l]
        cre = np.asarray(inp['s5_c_re'], f)[l]; cim = np.asarray(inp['s5_c_im'], f)[l]
        for gq in range(16):
            i, gg = gq // 2, gq % 2
            r0 = 32 * (i % 4) + 16 * gg
            s5pad[l, 3, r0:r0 + 16, i, 64 * gg:64 * gg + 64] = bre[gq].T
            s5pad[l, 4, r0:r0 + 16, i, 64 * gg:64 * gg + 64] = bim[gq].T
            s5pad[l, 5, 64 * gg:64 * gg + 64, i, r0:r0 + 16] = cre[gq].T
            s5pad[l, 6, 64 * gg:64 * gg + 64, i, r0:r0 + 16] = cim[gq].T
    gluw = np.ascontiguousarray(np.asarray(inp['s5_glu_w'], f).reshape(L, 2, 128, 256).transpose(0, 2, 1, 3))
    return {
        'win': win.reshape(L, NB_IN, 128, 1024), 'wout': wout, 'wup': wup.reshape(L, NB_UP, 128, 1024), 'wdn': wdn,
        'pp128': pp128, 'pp96': pp96, 'pp64': pp64, 's5pad': s5pad.reshape(L, 7, 128, 1024), 'gluw': gluw,
    }


_CACHE = {}


def kernel(**inputs):
    x = np.asarray(inputs['x'], np.float32)
    B, T, _ = x.shape
    L = np.asarray(inputs['w_in']).shape[0]
    key = (T, L)
    if key not in _CACHE:
        _CACHE[key] = build(T=T, L=L)
    nc = _CACHE[key]
    shared = prep_shared(inputs, L)
    in_maps = []
    for b in range(B):
        m = dict(shared)
        m['x'] = np.ascontiguousarray(x[b])
        in_maps.append(m)
    res = run_bass_kernel_spmd(nc, in_maps, core_ids=list(range(B)))
    return np.stack([np.asarray(r['out'], np.float32) for r in res.results], axis=0)
```

```python
import math
import numpy as np
from contextlib import ExitStack
import concourse.bass as bass
import concourse.mybir as mybir
from concourse.bass_utils import run_bass_kernel_spmd

F32 = mybir.dt.float32
BF16 = mybir.dt.bfloat16
I32 = mybir.dt.int32
AF = mybir.ActivationFunctionType
ALU = mybir.AluOpType

D = 1024
NIN = 2952
DFF = 2816
TG = 512
EPS = 1e-6
NB_UP = 44
NKD = 22
TWO_PI = 2.0 * math.pi

IN_BLOCKS = []
for b in range(3):
    IN_BLOCKS.append(('v', b, 768 + 128 * b, 128))
IN_BLOCKS.append(('if', 0, 1536, 8))
for h in range(4):
    IN_BLOCKS.append(('q', h, 96 * h, 96))
    IN_BLOCKS.append(('k', h, 384 + 96 * h, 96))
    IN_BLOCKS.append(('o', h, 1152 + 96 * h, 96))
for b in range(3):
    IN_BLOCKS.append(('sq', b, 1544 + 128 * b, 128))
for b in range(3):
    IN_BLOCKS.append(('sk', b, 1928 + 128 * b, 128))
for b in range(3):
    IN_BLOCKS.append(('sv', b, 2312 + 128 * b, 128))
for b in range(2):
    IN_BLOCKS.append(('u', b, 2696 + 128 * b, 128))
NB_IN = len(IN_BLOCKS)

PC_GMIX = 0
PC_GFFN = 8
PC_SBQG = 16
PC_SBKG = 17
PC_GLUB = 18
PC_S5G = 20
PC_S5D = 22
PC_FCW = 24
PC_GATEB = 90
PC_ARE = 98
PC_AIM = 106
PC_LDT = 114
PC_N = 122
Q_CW = 0
Q_CB = 32
Q_OG = 40
Q_N = 44


class KB:
    ALIAS = {}

    def __init__(self, nc, ctx, n_dma_sems=40):
        self.nc = nc
        self.ctx = ctx
        self.E = {'pe': nc.tensor, 'act': nc.scalar, 'dve': nc.vector, 'pool': nc.gpsimd, 'sp': nc.sync}
        self.sem = {}
        self.cnt = {}
        for e in self.E:
            self.sem[e] = ctx.enter_context(nc.semaphore("s_" + e))
            self.cnt[e] = 0
        self.dsem = [ctx.enter_context(nc.semaphore("d%d" % i)) for i in range(n_dma_sems)]
        self.dcnt = [0] * n_dma_sems
        self.dnext = 0
        self.seen = {e: {} for e in self.E}
        self.lastw = {}
        self.readers = {}
        self.nwaits = 0
        self.nins = 0
        self.inflight = {}

    def sb(self, name, shape, dt=F32):
        return self.ctx.enter_context(self.nc.sbuf_tensor(name, list(shape), dt))

    def ps(self, name, shape, dt=F32):
        return self.ctx.enter_context(self.nc.psum_tensor(name, list(shape), dt))

    @staticmethod
    def keys(aps):
        out = []
        for a in aps:
            if a is None or isinstance(a, (int, float)):
                continue
            if isinstance(a, (str, tuple)):
                out.append(a)
            elif id(a) in KB.ALIAS:
                out.append(KB.ALIAS[id(a)])
            else:
                out.append(a.name)
        return out

    def _wait(self, e, toks):
        best = {}
        for (sid, val) in toks:
            if sid == 'pe' and e == 'pe':
                continue
            if best.get(sid, 0) < val:
                best[sid] = val
        for sid, val in best.items():
            if self.seen[e].get(sid, 0) >= val:
                continue
            sem = self.sem[sid] if isinstance(sid, str) else self.dsem[sid]
            self.E[e].wait_ge(sem, val)
            self.seen[e][sid] = val
            self.nwaits += 1

    def _deps(self, r, w):
        toks = set()
        for k in r:
            if k in self.lastw:
                toks.add(self.lastw[k])
        for k in w:
            if k in self.lastw:
                toks.add(self.lastw[k])
            for t in self.readers.get(k, {}).items():
                toks.add(t)
        return toks

    def _record(self, tok, r, w):
        for k in r:
            d = self.readers.setdefault(k, {})
            if d.get(tok[0], 0) < tok[1]:
                d[tok[0]] = tok[1]
        for k in w:
            self.lastw[k] = tok
            self.readers[k] = {}

    def op(self, e, fn, *args, outs=(), ins=(), inc=True, **kw):
        r = self.keys(ins)
        w = self.keys(outs)
        self._wait(e, self._deps(r, w))
        ins_ = fn(*args, **kw)
        self.nins += 1
        if inc:
            self.cnt[e] += 1
            ins_.then_inc(self.sem[e], 1)
            tok = (e, self.cnt[e])
        else:
            tok = (e, self.cnt[e] + 1)
        self._record(tok, r, w)
        return ins_

    def dma(self, q, out, in_, ok=None, ik=None, **kw):
        j = self.dnext
        self.dnext = (self.dnext + 1) % len(self.dsem)
        r = self.keys([in_ if ik is None else ik])
        w = self.keys([out if ok is None else ok])
        toks = self._deps(r, w)
        if self.dcnt[j] > 0:
            toks.add((j, self.dcnt[j] * 16))
        fl = self.inflight.setdefault(q, [])
        lim = 4 if q == 'pool' else 12
        if len(fl) >= lim:
            toks.add(fl.pop(0))
        self._wait(q, toks)
        ins_ = self.E[q].dma_start(out=out, in_=in_, **kw)
        self.nins += 1
        self.dcnt[j] += 1
        ins_.then_inc(self.dsem[j], 16)
        tok = (j, self.dcnt[j] * 16)
        fl.append(tok)
        self._record(tok, r, w)
        return tok

    def barrier(self):
        toks = set()
        for e in self.E:
            if self.cnt[e] > 0:
                toks.add((e, self.cnt[e]))
        for j, c in enumerate(self.dcnt):
            if c > 0:
                toks.add((j, c * 16))
        for e in self.E:
            self._wait(e, toks)

    def wait_keys(self, e, aps):
        toks = set()
        for k in self.keys(aps):
            if k in self.lastw:
                toks.add(self.lastw[k])
        self._wait(e, toks)

    def mm(self, out, lhsT, rhs, start=True, stop=True, inc=True, **kw):
        return self.op('pe', self.nc.tensor.matmul, out, lhsT, rhs, start=start, stop=stop,
                       outs=[out], ins=[lhsT, rhs], inc=inc, **kw)

    def tr(self, out, in_, ident):
        return self.op('pe', self.nc.tensor.transpose, out, in_, ident, outs=[out], ins=[in_, ident])

    def act(self, out, in_, func, bias=None, scale=None, accum=None):
        kw = {}
        if bias is not None:
            kw['bias'] = bias
        if scale is not None:
            kw['scale'] = scale
        if accum is not None:
            kw['accum_out'] = accum
        return self.op('act', self.nc.scalar.activation, out, in_, func, outs=[out, accum],
                       ins=[in_, bias, scale], **kw)

    def tt(self, e, out, a, b, op):
        return self.op(e, self.E[e].tensor_tensor, out, a, b, op, outs=[out], ins=[a, b])

    def ts(self, e, out, a, s1, s2=None, op0=ALU.mult, op1=None):
        if op1 is None:
            return self.op(e, self.E[e].tensor_scalar, out, a, s1, None, op0, outs=[out], ins=[a, s1])
        return self.op(e, self.E[e].tensor_scalar, out, a, s1, s2, op0, op1, outs=[out], ins=[a, s1, s2])

    def stt(self, out, a, scalar, b, op0, op1):
        return self.op('dve', self.nc.vector.scalar_tensor_tensor, out, a, scalar, b, op0, op1,
                       outs=[out], ins=[a, scalar, b])

    def cp(self, e, out, in_):
        return self.op(e, self.E[e].tensor_copy, out, in_, outs=[out], ins=[in_])

    def ms(self, e, out, val):
        return self.op(e, self.E[e].memset, out, val, outs=[out], ins=[])

    def rcp(self, out, in_):
        return self.op('dve', self.nc.vector.reciprocal, out, in_, outs=[out], ins=[in_])

    def scan(self, out, d0, d1, init):
        return self.op('dve', self.nc.vector.tensor_tensor_scan, out, d0, d1, init, ALU.mult, ALU.add,
                       outs=[out], ins=[d0, d1, init])

    def asel(self, out, in_, pattern, cmp, fill, base, cm):
        return self.op('pool', self.nc.gpsimd.affine_select, out, in_, pattern=pattern, compare_op=cmp,
                       fill=fill, base=base, channel_multiplier=cm, outs=[out], ins=[in_])


def build(T=4096, L=2, enable=('ml', 'sb', 's5', 'ffn'), precast=True):
    import os
    precast = precast and not os.environ.get('NOPRECAST')
    DBG = int(os.environ.get('DBG', 99))
    NG = T // TG
    NT = T // 128
    nc = bass.Bass("TRN2", target_bir_lowering=False)
    dr = lambda name, shape, dt=F32, kind="ExternalInput": nc.dram_tensor(name, list(shape), dt, kind=kind).ap()
    x_d = dr("x", [T, D])
    win_d = dr("win", [L, NB_IN, 128, 1024])
    wout_d = dr("wout", [L, 12, 128, 1024])
    wup_d = dr("wup", [L, NB_UP, 128, 1024])
    wdn_d = dr("wdn", [L, NKD, 128, 1024])
    pp128_d = dr("pp128", [128, L, PC_N])
    pp96_d = dr("pp96", [96, L, Q_N])
    pp64_d = dr("pp64", [64, L, 6])
    s5pad_d = dr("s5pad", [L, 7, 128, 1024])
    gluw_d = dr("gluw", [L, 128, 2, 256])
    out_d = dr("out", [T, D], F32, "ExternalOutput")
    win_b = dr("win_b", [L, NB_IN, 128, 1024], BF16, "Internal")
    wout_b = dr("wout_b", [L, 12, 128, 1024], BF16, "Internal")
    wup_b = dr("wup_b", [L, NB_UP, 128, 1024], BF16, "Internal")
    wdn_b = dr("wdn_b", [L, NKD, 128, 1024], BF16, "Internal")

    DBGOUT = bool(os.environ.get('DBGOUT'))
    if DBGOUT:
        dbg_d = dr("dbg", [16, 128, 512], F32, "ExternalOutput")
    with ExitStack() as ctx:
        k = KB(nc, ctx)
        sb, ps = k.sb, k.ps

        def dbg(slot, ap, n):
            if DBGOUT:
                k.dma('sp', dbg_d[slot, 0:ap.shape[0], 0:n], ap, ok=('dbg', slot))
        ident = sb("ident", [128, 128])
        ones = sb("ones", [128, 128])
        onesbd = sb("onesbd", [128, 128])
        tri_le = sb("tri_le", [128, 128])
        negmask = sb("negmask", [128, 128])
        negU8 = sb("negU8", [128, 128], BF16)
        neg8 = sb("neg8", [128, 128], BF16)
        selneg = sb("selneg", [4, 4, 128])
        sbmask = sb("sbmask", [128, 4, 512], BF16)
        jrow = sb("jrow", [128, 128])
        k.ms('pool', ident[:], 0.0)
        k.asel(ident[:], ident[:], [[-1, 128]], ALU.not_equal, 1.0, 0, 1)
        k.ms('pool', ones[:], 1.0)
        k.ms('dve', onesbd[:], 0.0)
        k.ms('dve', onesbd[0:64, 0:64], 1.0)
        k.ms('dve', onesbd[64:128, 64:128], 1.0)
        k.ms('pool', tri_le[:], 1.0)
        k.asel(tri_le[:], tri_le[:], [[1, 128]], ALU.is_ge, 0.0, 0, -1)
        k.ms('pool', negmask[:], 0.0)
        k.asel(negmask[:], negmask[:], [[1, 128]], ALU.is_ge, -30000.0, 0, -1)
        k.ms('pool', negU8[:], -8.0)
        k.asel(negU8[:], negU8[:], [[-1, 128]], ALU.is_ge, 0.0, 0, 1)
        k.ms('pool', neg8[:], -8.0)
        k.ms('pool', selneg[:], 0.0)
        k.asel(selneg[:], selneg[:], [[-1, 4], [0, 128]], ALU.not_equal, -1.0, 0, 1)
        k.ms('pool', sbmask[:], 1.0)
        for j in range(4):
            k.asel(sbmask[:, j, :], sbmask[:, j, :], [[1, 512]], ALU.is_gt, 0.0, -128 * j, -1)
        jrow_i = sb("jrow_i", [128, 128], I32)
        k.op('pool', nc.gpsimd.iota, jrow_i[:], pattern=[[1, 128]], base=1, channel_multiplier=0,
             outs=[jrow_i], ins=[])
        k.cp('dve', jrow[:], jrow_i[:])

        pp128 = sb("pp128s", [128, L, PC_N])
        pp96 = sb("pp96s", [96, L, Q_N])
        pp64 = sb("pp64s", [64, L, 6])
        k.dma('sp', pp128[:], pp128_d)
        k.dma('sp', pp96[:], pp96_d)
        k.dma('sp', pp64[:], pp64_d)

        for l in range(L if precast else 0):
            for (src, dst, n) in ((win_d, win_b, NB_IN), (wout_d, wout_b, 12), (wup_d, wup_b, NB_UP), (wdn_d, wdn_b, NKD)):
                for b in range(n):
                    k.dma('pool', dst[l, b], src[l, b], ok=(dst.name, l, b))

        KT = sb("KT", [128, 3, T], BF16)
        Vr = sb("Vr", [128, NT, 384], BF16)
        Cst = [sb("Cst%d" % h, [96, 192]) for h in range(4)]
        s5st = sb("s5st", [128, 8, 2])
        halo_ml = sb("halo_ml", [96, 8, 3])
        halo_ff = sb("halo_ff", [128, NKD, 2])
        Bre = sb("Bre", [128, 8, 128]); Bim = sb("Bim", [128, 8, 128])
        Cre = sb("Cre", [128, 8, 128]); Cimn = sb("Cimn", [128, 8, 128])
        iti = sb("iti", [128, 256], I32)
        CS = sb("CS", [128, 8, 2, 128])
        s5r = sb("s5r", [128, 8])
        s5sm = [sb("s5sm%d" % i, [128, 8]) for i in range(6)]
        gluw = sb("gluws", [128, 2, 256])
        xt = [sb("xt%d" % i, [128, D]) for i in range(2)]
        xs = sb("xs", [128, D])
        ssum = sb("ssum", [128, 4]); rstd = sb("rstd", [128, 4])
        hT = sb("hT", [128, 8, TG], BF16)
        mix_ml = sb("mix_ml", [96, 4, TG], BF16)
        mix_sb = sb("mix_sb", [64, 6, TG], BF16)
        mix_s5 = sb("mix_s5", [128, 2, TG], BF16)
        NWP = 4
        wp = [sb("wp%d" % i, [128, 1024], BF16) for i in range(NWP)]
        wpi = [0]
        vtok = sb("vtok", [128, 4, 384])
        gps = sb("gps", [128, 4, 8])
        gi = sb("gi", [128, 4, 4]); spf = sb("spf", [128, 4, 4]); gtmp = sb("gtmp", [128, 4, 4])
        bcum = sb("bcum", [128, 4]); dcol = sb("dcol", [128, 4]); wcol = sb("wcol", [128, 4])
        bcumT = sb("bcumT", [4, 128])
        qT = sb("qT", [96, TG]); kT = sb("kT", [96, TG]); osig = sb("osig", [96, TG]); hml = sb("hml", [96, TG])
        et = [sb("et%d" % i, [128, 128]) for i in range(2)]
        pt = [sb("pt%d" % i, [128, 128]) for i in range(2)]
        ebq = [sb("ebq%d" % i, [96, 128]) for i in range(2)]
        qd = [sb("qd%d" % i, [96, 128]) for i in range(2)]
        dm = [sb("dm%d" % i, [96, 128]) for i in range(1)] * 2
        kw_ = [sb("kw%d" % i, [128, 96]) for i in range(2)]
        t5 = [sb("t5_%d" % i, [128, TG]) for i in range(6)]
        t5i = [0]
        uT = sb("uT", [128, 2, TG])
        spb = [uT[:, 0, :].bitcast(BF16)[:, 0:TG], uT[:, 0, :].bitcast(BF16)[:, TG:2 * TG]]
        spaccb = [uT[:, 1, :].bitcast(BF16)[:, 0:TG], uT[:, 1, :].bitcast(BF16)[:, TG:2 * TG]]
        KB.ALIAS.clear()
        for i_, v_ in enumerate(spb + spaccb):
            KB.ALIAS[id(v_)] = 'attn_bf%d' % i_
        build.keep = spb + spaccb
        sq_bf = sb("sq_bf", [128, 3, TG], BF16)
        sp_sb = [sb("sp_sb%d" % i, [128, TG]) for i in range(2)]
        spacc = [sb("spacc%d" % i, [128, TG]) for i in range(2)]
        at_sb = [sb("at_sb%d" % i, [128, TG], BF16) for i in range(2)]
        aT = sb("aT", [128, NKD, TG], BF16)
        gst = [sb("gst%d" % i, [128, TG + 3]) for i in range(2)]
        stg = [g_[0:96, :] for g_ in gst]
        pb = [ps("pb%d" % i, [128, 512]) for i in range(8)]
        rot = [0]

        def nps(lst=(0, 1, 2, 3, 4)):
            rot[0] = (rot[0] + 1) % len(lst)
            return pb[lst[rot[0]]]

        def ntmp():
            t5i[0] = (t5i[0] + 1) % len(t5)
            return t5[t5i[0]]

        f32src = {win_b.name: win_d, wout_b.name: wout_d, wup_b.name: wup_d, wdn_b.name: wdn_d}

        def load_w(src, src_key=None):
            b = wp[wpi[0]]
            wpi[0] = (wpi[0] + 1) % NWP
            if precast:
                k.dma('sp', b[:], src, ik=src_key)
            else:
                k.dma('pool', b[:], f32src[src_key[0]][src_key[1], src_key[2]])
            return b

        def norm_T(l, g, src_d, gcol, src_name):
            t0 = g * TG
            for j in range(4):
                xb = xt[j % 2]
                k.dma('sp', xb[:], src_d[t0 + 128 * j: t0 + 128 * (j + 1), :], ik=(src_name, 4 * g + j))
                k.act(xs[:], xb[:], AF.Square, accum=ssum[:, j:j + 1])
                k.ts('dve', rstd[:, j:j + 1], ssum[:, j:j + 1], 1.0 / D, EPS, ALU.mult, ALU.add)
                k.act(rstd[:, j:j + 1], rstd[:, j:j + 1], AF.Sqrt)
                k.rcp(rstd[:, j:j + 1], rstd[:, j:j + 1])
                k.ts('dve', xs[:], xb[:], rstd[:, j:j + 1], None, ALU.mult)
                for half in range(2):
                    p = nps()
                    for c in range(4):
                        kc = half * 4 + c
                        k.tr(p[:, 128 * c:128 * (c + 1)], xs[:, 128 * kc:128 * (kc + 1)], ident[:])
                    gb = pp128[:, l, gcol + 4 * half: gcol + 4 * half + 4].unsqueeze(2).to_broadcast([128, 4, 128])
                    k.tt('dve', hT[:, 4 * half:4 * half + 4, 128 * j:128 * (j + 1)],
                         p[:].rearrange("p (c t) -> p c t", c=4), gb, ALU.mult)

        def proj_fm(wb, M, p):
            wv = wb[:].rearrange("p (k c) -> p k c", k=8)
            for kc in range(8):
                k.mm(p[0:M, :], wv[:, kc, 0:M], hT[:, kc, :], start=(kc == 0), stop=(kc == 7), inc=(kc == 7))

        def proj_tm(wb, ncols, p):
            wv = wb[:].rearrange("p (k c) -> p k c", k=8)
            pv = p[:].rearrange("p (j c) -> p j c", j=4)
            for j in range(4):
                for kc in range(8):
                    k.mm(pv[:, j, 0:ncols], hT[:, kc, 128 * j:128 * (j + 1)], wv[:, kc, 0:ncols],
                         start=(kc == 0), stop=(kc == 7), inc=(kc == 7))
            return pv

        def rms_fm(src_sb, sq_src, n, onesm, out_ap, gscalar, extra=None):
            sq = ntmp()
            k.act(sq[0:n, :], sq_src, AF.Square)
            p = nps()
            k.mm(p[0:n, :], onesm, sq[0:n, :])
            r = ntmp()
            k.ts('dve', r[0:n, :], p[0:n, :], 1.0 / (n if n != 128 else 64), EPS, ALU.mult, ALU.add)
            k.act(r[0:n, :], r[0:n, :], AF.Sqrt)
            k.rcp(r[0:n, :], r[0:n, :])
            if extra is None:
                k.stt(out_ap, src_sb, gscalar, r[0:n, :], ALU.mult, ALU.mult)
            else:
                k.tt('dve', r[0:n, :], r[0:n, :], extra, ALU.mult)
                k.stt(out_ap, src_sb, gscalar, r[0:n, :], ALU.mult, ALU.mult)

        def lam_bar(are, aim, ldt, F, o_re, o_im, o_cr, o_ci, tmp):
            t0_, t1_, t2_, ti = tmp
            k.ts('dve', are, are, -1e-4, None, ALU.min)
            k.act(ldt, ldt, AF.Exp)
            k.tt('dve', t0_, are, ldt, ALU.mult)
            k.act(t0_, t0_, AF.Exp)
            k.tt('dve', t1_, aim, ldt, ALU.mult)

            def sin_of(dst, src, shift):
                k.ts('dve', t2_, src, 1.0 / TWO_PI, shift / TWO_PI, ALU.mult, ALU.add)
                k.cp('dve', ti, t2_)
                k.cp('dve', t2_, ti)
                k.ts('dve', t2_, t2_, -TWO_PI, shift, ALU.mult, ALU.add)
                k.tt('dve', t2_, t2_, src, ALU.add)
                k.act(dst, t2_, AF.Sin)
            sin_of(o_im, t1_, 0.0)
            sin_of(o_re, t1_, math.pi / 2)
            k.tt('dve', o_re, o_re, t0_, ALU.mult)
            k.tt('dve', o_im, o_im, t0_, ALU.mult)
            if o_cr is None:
                return
            k.tt('dve', t0_, are, are, ALU.mult)
            k.tt('dve', t1_, aim, aim, ALU.mult)
            k.tt('dve', t0_, t0_, t1_, ALU.add)
            k.rcp(t0_, t0_)
            k.ts('dve', t1_, o_re, -1.0, None, ALU.add)
            k.tt('dve', o_cr, t1_, are, ALU.mult)
            k.tt('dve', t2_, o_im, aim, ALU.mult)
            k.tt('dve', o_cr, o_cr, t2_, ALU.add)
            k.tt('dve', o_cr, o_cr, t0_, ALU.mult)
            k.tt('dve', o_ci, o_im, are, ALU.mult)
            k.tt('dve', t2_, t1_, aim, ALU.mult)
            k.tt('dve', o_ci, o_ci, t2_, ALU.subtract)
            k.tt('dve', o_ci, o_ci, t0_, ALU.mult)

        def s5_setup(l):
            f = lambda t: t[:].rearrange("p a b -> p (a b)")
            k.dma('sp', f(Cre), s5pad_d[l, 5]); k.dma('sp', f(Cimn), s5pad_d[l, 6])
            k.dma('sp', gluw[:], gluw_d[l])
            k.ts('dve', f(Cimn), f(Cimn), -1.0, None, ALU.mult)
            for hf in range(4):
                cs = slice(256 * hf, 256 * (hf + 1))
                H = slice(0, 256)
                a0, a1, a2, lbr, lbi, T0, T1, T2 = t5[0][:, H], t5[1][:, H], t5[2][:, H], t5[3][:, H], t5[4][:, H], t5[5][:, H], sp_sb[0][:, H], sp_sb[1][:, H]
                cr, ci = spacc[0][:, H], spacc[1][:, H]
                k.dma('sp', a0, s5pad_d[l, 0][:, cs]); k.dma('sp', a1, s5pad_d[l, 1][:, cs])
                k.dma('sp', a2, s5pad_d[l, 2][:, cs])
                k.dma('sp', f(Bre)[:, cs], s5pad_d[l, 3][:, cs]); k.dma('sp', f(Bim)[:, cs], s5pad_d[l, 4][:, cs])
                lam_bar(a0, a1, a2, 256, lbr, lbi, cr, ci, [T0, T1, T2, iti[:]])
                k.tt('dve', T0, cr, f(Bre)[:, cs], ALU.mult)
                k.tt('dve', T1, ci, f(Bim)[:, cs], ALU.mult)
                k.tt('dve', T0, T0, T1, ALU.subtract)
                k.tt('dve', T1, cr, f(Bim)[:, cs], ALU.mult)
                k.tt('dve', T2, ci, f(Bre)[:, cs], ALU.mult)
                k.tt('dve', f(Bim)[:, cs], T1, T2, ALU.add)
                k.cp('dve', f(Bre)[:, cs], T0)
            are = s5sm[0]; aim = s5sm[1]; ldt = s5sm[2]
            k.cp('dve', are[:], pp128[:, l, PC_ARE:PC_ARE + 8])
            k.cp('dve', aim[:], pp128[:, l, PC_AIM:PC_AIM + 8])
            k.cp('dve', ldt[:], pp128[:, l, PC_LDT:PC_LDT + 8])
            k.ts('dve', are[:], are[:], -1e-4, None, ALU.min)
            k.act(ldt[:], ldt[:], AF.Exp)
            k.tt('dve', s5r[:], are[:], ldt[:], ALU.mult)
            k.act(s5r[:], s5r[:], AF.Exp)
            th = s5sm[3]
            k.tt('dve', th[:], aim[:], ldt[:], ALU.mult)
            fh = s5sm[4]; fl = s5sm[5]
            k.ts('dve', fh[:], th[:], 2048.0 / TWO_PI, None, ALU.mult)
            k.cp('dve', iti[:, 0:8], fh[:])
            k.cp('dve', fh[:], iti[:, 0:8])
            k.ts('dve', fh[:], fh[:], 1.0 / 2048.0, None, ALU.mult)
            k.ts('dve', fl[:], th[:], 1.0 / TWO_PI, None, ALU.mult)
            k.tt('dve', fl[:], fl[:], fh[:], ALU.subtract)
            tA = t5[0][:, 0:128]; tB = t5[1][:, 0:128]; tI = iti[:, 0:128]
            for i in range(8):
                for (dsti, shift) in ((1, 0.0), (0, 0.25)):
                    k.ts('dve', tA, jrow[:], fh[:, i:i + 1], None, ALU.mult)
                    k.cp('dve', tI, tA)
                    k.cp('dve', tB, tI)
                    k.tt('dve', tA, tA, tB, ALU.subtract)
                    k.stt(tA, jrow[:], fl[:, i:i + 1], tA, ALU.mult, ALU.add)
                    if shift != 0.0:
                        k.ts('dve', tA, tA, shift, None, ALU.add)
                    k.cp('dve', tI, tA)
                    k.cp('dve', tB, tI)
                    k.tt('dve', tA, tA, tB, ALU.subtract)
                    k.act(CS[:, i, dsti, :], tA, AF.Sin, scale=TWO_PI)
            if l == 0:
                dbg(0, Bre[:, 0, :], 128); dbg(1, Bim[:, 0, :], 128); dbg(2, Bre[:, 5, :], 128)
                pass
                dbg(7, s5r[:], 8)

        def mlstm_head(l, g, h):
            def SA(j):
                cs = slice(128 * j, 128 * (j + 1))
                b = j % 2
                pB = nps()
                k.mm(pB[:, 0:128], selneg[:, h, :], bcumT_all[j][:, :], start=True, stop=False)
                k.mm(pB[:, 0:128], ident[:], negmask[:], start=False, stop=True)
                k.act(et[b][:], pB[:, 0:128], AF.Exp, bias=dcol_all[:, j, h:h + 1])
                k.mm(pB[0:96, 128:256], selneg[:, h, 0:96], bcumT_all[j][:, :])
                k.act(ebq[b][:], pB[0:96, 128:256], AF.Exp)
                k.tt('dve', qd[b][:], qT[:, cs], ebq[b][:], ALU.mult)
                pS = nps()
                k.mm(pS[:, 0:128], kT[:, cs], qT[:, cs])
                k.tt('dve', pt[b][:], pS[:, 0:128], et[b][:], ALU.mult)
                k.tr(pS[:, 128:224], kT[:, cs], ident[0:96, 0:96])
                k.ts('dve', kw_[b][:], pS[:, 128:224], wcol_all[:, j, h:h + 1], None, ALU.mult)

            def SB(j):
                cs = slice(128 * j, 128 * (j + 1))
                b = j % 2
                pN = nps()
                k.mm(pN[0:96, 0:128], vtok[:, j, 96 * h:96 * (h + 1)], pt[b][:], start=True, stop=False)
                k.mm(pN[0:96, 0:128], Cst[h][:, 0:96], qd[b][:], start=False, stop=True)
                k.mm(pN[0:96, 128:256], ones[:, 0:96], pt[b][:], start=True, stop=False)
                k.mm(pN[0:96, 128:256], Cst[h][:, 96:192], qd[b][:], start=False, stop=True)
                k.mm(pN[0:96, 256:352], kw_[b][:], vtok[:, j, 96 * h:96 * (h + 1)])
                k.mm(pN[0:96, 352:448], kw_[b][:], ones[:, 0:96])
                k.stt(Cst[h][:], Cst[h][:], ebq[b][:, 127:128], pN[0:96, 256:448], ALU.mult, ALU.add)
                k.ts('dve', dm[b][:], pN[0:96, 128:256], 1.0, None, ALU.max)
                k.stt(dm[b][:], pN[0:96, 128:256], -1.0, dm[b][:], ALU.mult, ALU.max)
                k.rcp(dm[b][:], dm[b][:])
                k.tt('dve', hml[:, cs], pN[0:96, 0:128], dm[b][:], ALU.mult)

            SA(0)
            for j in range(4):
                if j + 1 < 4:
                    SA(j + 1)
                SB(j)
            rms_fm(hml[:], hml[:], 96, ones[0:96, 0:96], mix_ml[:, h, :], pp96[:, l, Q_OG + h:Q_OG + h + 1], extra=osig[:])

        def gates_prep(l):
            gb = pp128[:, l, PC_GATEB:PC_GATEB + 8]
            k.tt('dve', gi[:], gps[:, :, 0:4], gb[:, 0:4].unsqueeze(1).to_broadcast([128, 4, 4]), ALU.add)
            k.ts('dve', gi[:], gi[:], -0.5 * math.log(96.0), None, ALU.add)
            k.tt('dve', gtmp[:], gps[:, :, 4:8], gb[:, 4:8].unsqueeze(1).to_broadcast([128, 4, 4]), ALU.add)
            k.act(gtmp[:], gtmp[:], AF.Exp, scale=-1.0)
            k.act(spf[:], gtmp[:], AF.Ln, bias=1.0)
            for j in range(4):
                p = nps()
                k.mm(p[:, 0:4], tri_le[:], spf[:, j, :])
                k.mm(p[:, 4:8], ones[:], spf[:, j, :])
                k.mm(p[0:4, 128:256], spf[:, j, :], tri_le[:])
                k.tt('dve', dcol_all[:, j, :], gi[:, j, :], p[:, 0:4], ALU.add)
                k.tt('dve', wcol_all[:, j, :], dcol_all[:, j, :], p[:, 4:8], ALU.subtract)
                k.act(wcol_all[:, j, :], wcol_all[:, j, :], AF.Exp)
                k.cp('dve', bcumT_all[j][:, :], p[0:4, 128:256])

        dcol_all = sb("dcol_all", [128, 4, 4]); wcol_all = sb("wcol_all", [128, 4, 4])
        bcumT_all = [sb("bcumT%d" % j, [4, 128]) for j in range(4)]

        def sb_attention(l, g):
            for h in range(6):
                pr = h // 2
                lo = 64 * (h % 2)
                qh = sq_bf[lo:lo + 64, pr, :]
                O = pb[7]
                kbs = list(range(4 * g + 3, -1, -1))
                n = len(kbs)

                def kt_of(idx):
                    kb = kbs[idx]
                    return KT[lo:lo + 64, pr, 128 * kb:128 * (kb + 1)]

                def S1(idx):
                    b = idx % 2
                    kb = kbs[idx]
                    Z = pb[idx % 4]
                    k.mm(Z[:], kt_of(idx), qh, start=True, stop=False)
                    k.act(sp_sb[b][:], Z[:], AF.Exp, scale=0.125)
                    k.act(spb[b], sp_sb[b][:], AF.Ln, bias=1.0)
                    if kb >= 4 * g:
                        k.tt('dve', spb[b], spb[b], sbmask[:, kb - 4 * g, :], ALU.mult)

                def S2(idx):
                    b = idx % 2
                    kb = kbs[idx]
                    A = pb[idx % 4]
                    k.mm(A[:], negU8[:], spb[b], start=False, stop=(idx == 0))
                    if idx > 0:
                        k.mm(A[:], neg8[:], spaccb[1 - b], start=False, stop=True)
                        if idx + 1 < n:
                            k.tt('dve', spacc[b][:], spacc[1 - b][:], spb[b], ALU.add)
                            k.cp('dve', spaccb[b], spacc[b][:])
                    else:
                        k.cp('dve', spacc[b][:], spb[b])
                        k.cp('dve', spaccb[b], spb[b])
                    k.act(at_sb[b][:], A[:], AF.Exp, scale=0.125)
                    if kb >= 4 * g:
                        k.tt('dve', at_sb[b][:], at_sb[b][:], sbmask[:, kb - 4 * g, :], ALU.mult)

                def S3(idx):
                    b = idx % 2
                    kb = kbs[idx]
                    k.mm(O[0:64, :], Vr[:, kb, 64 * h:64 * (h + 1)], at_sb[b][:], start=(idx == 0), stop=(idx == n - 1))

                S1(0)
                for idx in range(n):
                    if idx + 1 < n:
                        S1(idx + 1)
                    S2(idx)
                    S3(idx)
                osb = ntmp()
                k.act(osb[0:64, :], O[0:64, :], AF.Copy)
                rms_fm(osb[0:64, :], O[0:64, :], 64, ones[0:64, 0:64], mix_sb[:, h, :], pp64[:, l, h:h + 1])

        def s5_group(l, g):
            Y = [pb[5], pb[6]]
            W = 128
            T2 = hT[:].rearrange("p a b -> p (a b)").bitcast(F32).rearrange("p (i c w) -> p i c w", i=8, c=2)
            k.cp('pool', T2[:, :, 0, :], CS[:, :, 1, :])
            k.ts('pool', T2[:, :, 1, :], CS[:, :, 0, :], -1.0, None, ALU.mult)
            its = [(sc, i) for sc in range(4) for i in range(8)]
            banks = {}
            bufs = {}

            def P(n):
                sc, i = its[n]
                c = i // 4
                hs = slice(W * sc, W * (sc + 1))
                pR = nps()
                banks[n] = pR
                k.mm(pR[:, 0:W], Bre[:, i, :], uT[:, c, hs])
                k.mm(pR[:, W:2 * W], Bim[:, i, :], uT[:, c, hs])

            def V(n):
                sc, i = its[n]
                pR = banks[n]
                bA = ntmp(); bB = ntmp(); bC = ntmp()
                v3 = lambda ap: ap.rearrange("p (c w) -> p c w", c=2)
                bc = lambda ap: ap.unsqueeze(1).to_broadcast([128, 2, W])
                t1 = bA[:, 0:2 * W]; t2 = bA[:, 2 * W:4 * W]
                m = bB[:, 0:2 * W]; sr_ = bB[:, 2 * W:3 * W]; sm_ = bB[:, 3 * W:4 * W]
                u1 = bC[:, 0:2 * W]; u2 = bC[:, 2 * W:4 * W]
                so = bA[:, 0:2 * W]
                k.tt('dve', v3(t1), bc(pR[:, 0:W]), CS[:, i], ALU.mult)
                k.tt('dve', v3(t2), bc(pR[:, W:2 * W]), T2[:, i], ALU.mult)
                k.tt('dve', m, t1, t2, ALU.add)
                rb = s5r[:, i:i + 1].to_broadcast([128, W])
                k.scan(sr_, rb, m[:, 0:W], s5st[:, i, 0:1])
                k.scan(sm_, rb, m[:, W:2 * W], s5st[:, i, 1:2])
                k.tt('pool', v3(u1), bc(sr_), CS[:, i], ALU.mult)
                k.tt('pool', v3(u2), bc(sm_), T2[:, i], ALU.mult)
                k.tt('pool', so, u1, u2, ALU.add)
                k.cp('pool', s5st[:, i, 0:1], so[:, W - 1:W])
                k.ts('pool', s5st[:, i, 1:2], so[:, 2 * W - 1:2 * W], -1.0, None, ALU.mult)
                bufs[n] = so

            def C(n):
                sc, i = its[n]
                c = i // 4
                hs = slice(W * sc, W * (sc + 1))
                so = bufs[n]
                k.mm(Y[c][:, hs], Cre[:, i, :], so[:, 0:W], start=(i % 4 == 0), stop=False)
                k.mm(Y[c][:, hs], Cimn[:, i, :], so[:, W:2 * W], start=False, stop=(i % 4 == 3))

            P(0)
            for n in range(len(its)):
                if n + 1 < len(its):
                    P(n + 1)
                V(n)
                C(n)
            if l == 0 and g == 0:
                yd = ntmp()
                k.cp('dve', yd[:], Y[0][:]); dbg(8, yd[:], 512)
                yd2 = ntmp()
                k.cp('dve', yd2[:], Y[1][:]); dbg(14, yd2[:], 512)
                dbg(9, uT[:, 0, :], 512); dbg(13, uT[:, 1, :], 512)
            yv = [sp_sb[0], sp_sb[1]]
            for c in range(2):
                y = yv[c]
                k.stt(y[:], uT[:, c, :], pp128[:, l, PC_S5D + c:PC_S5D + c + 1], Y[c][:], ALU.mult, ALU.add)
                a = ntmp()
                k.tt('dve', a[:], y[:], y[:], ALU.mult)
                k.ts('dve', a[:], a[:], 0.044715, 1.0, ALU.mult, ALU.add)
                k.tt('dve', a[:], a[:], y[:], ALU.mult)
                k.act(a[:], a[:], AF.Sigmoid, scale=2.0 * math.sqrt(2.0 / math.pi))
                k.tt('dve', y[:], y[:], a[:], ALU.mult)
                if l == 0 and g == 0 and c == 0:
                    dbg(10, y[:], 512)
            sqs = []
            for c in range(2):
                p = nps()
                k.mm(p[:], gluw[:, 0, 128 * c:128 * (c + 1)], yv[0][:], start=True, stop=False)
                k.mm(p[:], gluw[:, 1, 128 * c:128 * (c + 1)], yv[1][:], start=False, stop=True)
                s = ntmp()
                k.act(s[:], p[:], AF.Sigmoid, bias=pp128[:, l, PC_GLUB + c:PC_GLUB + c + 1])
                sqs.append(s)
            for c in range(2):
                s = sqs[c]
                k.tt('dve', yv[c][:], yv[c][:], s[:], ALU.mult)
                if l == 0 and g == 0 and c == 0:
                    dbg(11, yv[c][:], 512)
                k.act(s[:], yv[c][:], AF.Square)
            p = nps()
            k.mm(p[:], ones[:], sqs[0][:], start=True, stop=False)
            k.mm(p[:], ones[:], sqs[1][:], start=False, stop=True)
            r = spacc[0]
            k.ts('dve', r[:], p[:], 1.0 / 256, EPS, ALU.mult, ALU.add)
            k.act(r[:], r[:], AF.Sqrt)
            k.rcp(r[:], r[:])
            if l == 0 and g == 0:
                dbg(12, r[:], 512)
            for c in range(2):
                k.stt(mix_s5[:, c, :], yv[c][:], pp128[:, l, PC_S5G + c:PC_S5G + c + 1], r[:], ALU.mult, ALU.mult)

        for h in range(4):
            k.ms('dve', Cst[h][:], 0.0)
        for l in range(L):
            src_d = x_d if l == 0 else out_d
            src_name = 'x' if l == 0 else 'out'
            if 's5' in enable:
                s5_setup(l)
            k.ms('dve', s5st[:], 0.0)
            k.ms('dve', halo_ml[:], 0.0)
            k.ms('dve', halo_ff[:], 0.0)
            for h in range(4):
                k.ms('dve', Cst[h][:], 0.0)
            for g in range(NG):
                t0 = g * TG
                norm_T(l, g, src_d, PC_GMIX, src_name)
                if 'ml' not in enable:
                    k.ms('pool', mix_ml[:], 0.0)
                if 'sb' not in enable:
                    k.ms('pool', mix_sb[:], 0.0)
                if 's5' not in enable:
                    k.ms('pool', mix_s5[:], 0.0)
                for bi_, (nm, idx, col0, ncols) in enumerate(IN_BLOCKS):
                    grp = {'v': 'ml', 'if': 'ml', 'q': 'ml', 'k': 'ml', 'o': 'ml', 'sq': 'sb', 'sk': 'sb', 'sv': 'sb', 'u': 's5'}[nm]
                    if grp not in enable:
                        continue
                    wb = load_w(win_b[l, bi_], (win_b.name, l, bi_))
                    p = nps()
                    if nm == 'v':
                        pv = proj_tm(wb, 128, p)
                        k.cp('dve', vtok[:, :, 128 * idx:128 * (idx + 1)], pv[:, :, :])
                    elif nm == 'if':
                        pv = proj_tm(wb, 8, p)
                        k.cp('dve', gps[:], pv[:, :, 0:8])
                        if DBG >= 1:
                            gates_prep(l)
                    elif nm in ('q', 'k'):
                        if DBG < 2:
                            continue
                        proj_fm(wb, 96, p)
                        blk = idx if nm == 'q' else 4 + idx
                        s_ = stg[bi_ % 2]
                        cv = ntmp()[0:96, :]
                        k.act(s_[:, 3:TG + 3], p[0:96, :], AF.Copy)
                        k.cp('dve', s_[:, 0:3], halo_ml[:, blk, :])
                        k.cp('dve', halo_ml[:, blk, :], s_[:, TG:TG + 3])
                        cw = pp96[:, l, Q_CW + 4 * blk:Q_CW + 4 * blk + 4]
                        k.ts('dve', cv, s_[:, 3:TG + 3], cw[:, 3:4], pp96[:, l, Q_CB + blk:Q_CB + blk + 1], ALU.mult, ALU.add)
                        for kk in range(3):
                            k.stt(cv, s_[:, kk:kk + TG], cw[:, kk:kk + 1], cv, ALU.mult, ALU.add)
                        k.act((qT if nm == 'q' else kT)[:], cv, AF.Silu)
                    elif nm == 'o':
                        if DBG < 3:
                            continue
                        proj_fm(wb, 96, p)
                        k.act(osig[:], p[0:96, :], AF.Sigmoid)
                        if DBG >= 4:
                            mlstm_head(l, g, idx)
                    elif nm in ('sq', 'sk'):
                        proj_fm(wb, 128, p)
                        raw = ntmp()
                        k.act(raw[:], p[:], AF.Copy)
                        gcol = PC_SBQG if nm == 'sq' else PC_SBKG
                        dst = sq_bf[:, idx, :] if nm == 'sq' else KT[:, idx, t0:t0 + TG]
                        rms_fm(raw[:], p[:], 128, onesbd[:], dst, pp128[:, l, gcol:gcol + 1])
                    elif nm == 'sv':
                        pv = proj_tm(wb, 128, p)
                        k.cp('dve', Vr[:, 4 * g:4 * g + 4, 128 * idx:128 * (idx + 1)], pv[:, :, :])
                        if idx == 2 and DBG >= 11:
                            sb_attention(l, g)
                    elif nm == 'u':
                        proj_fm(wb, 128, p)
                        k.act(uT[:, idx, :], p[:], AF.Copy)
                        if idx == 1:
                            s5_group(l, g)
                for b in range(12):
                    wb = load_w(wout_b[l, b], (wout_b.name, l, b))
                    for j in range(4):
                        cs = slice(128 * j, 128 * (j + 1))
                        if b < 4:
                            lhs = mix_ml[:, b, cs]; rows = 96
                        elif b < 10:
                            lhs = mix_sb[:, b - 4, cs]; rows = 64
                        else:
                            lhs = mix_s5[:, b - 10, cs]; rows = 128
                        for half in range(2):
                            k.mm(pb[2 * j + half][:], lhs, wb[0:rows, 512 * half:512 * (half + 1)],
                                 start=(b == 0), stop=(b == 11))
                for j in range(4):
                    xb = xt[j % 2]
                    k.dma('sp', xb[:], src_d[t0 + 128 * j: t0 + 128 * (j + 1), :], ik=(src_name, 4 * g + j))
                    for half in range(2):
                        k.tt('dve', xb[:, 512 * half:512 * (half + 1)], xb[:, 512 * half:512 * (half + 1)],
                             pb[2 * j + half][:], ALU.add)
                    k.dma('sp', out_d[t0 + 128 * j: t0 + 128 * (j + 1), :], xb[:], ok=('out', 4 * g + j))
                if 'ffn' not in enable:
                    continue
                norm_T(l, g, out_d, PC_GFFN, 'out')
                for b in range(NKD):
                    wg = load_w(wup_b[l, 2 * b], (wup_b.name, l, 2 * b))
                    wv_ = load_w(wup_b[l, 2 * b + 1], (wup_b.name, l, 2 * b + 1))
                    pg = nps(); pv_ = nps()
                    proj_fm(wg, 128, pg)
                    proj_fm(wv_, 128, pv_)
                    s_ = gst[b % 2]
                    k.act(s_[:, 2:TG + 2], pg[:], AF.Copy)
                    k.cp('dve', s_[:, 0:2], halo_ff[:, b, :])
                    k.cp('dve', halo_ff[:, b, :], s_[:, TG:TG + 2])
                    cw = pp128[:, l, PC_FCW + 3 * b:PC_FCW + 3 * b + 3]
                    c_ = ntmp()
                    k.ts('dve', c_[:], s_[:, 2:TG + 2], cw[:, 2:3], None, ALU.mult)
                    for kk in range(2):
                        k.stt(c_[:], s_[:, kk:kk + TG], cw[:, kk:kk + 1], c_[:], ALU.mult, ALU.add)
                    k.act(c_[:], c_[:], AF.Silu)
                    k.tt('dve', aT[:, b, :], c_[:], pv_[:], ALU.mult)
                for kc in range(NKD):
                    wd = load_w(wdn_b[l, kc], (wdn_b.name, l, kc))
                    for j in range(4):
                        for half in range(2):
                            k.mm(pb[2 * j + half][:], aT[:, kc, 128 * j:128 * (j + 1)], wd[:, 512 * half:512 * (half + 1)],
                                 start=(kc == 0), stop=(kc == NKD - 1), inc=(kc == NKD - 1 or (j == 3 and half == 1)))
                for j in range(4):
                    xb = xt[j % 2]
                    k.dma('sp', xb[:], out_d[t0 + 128 * j: t0 + 128 * (j + 1), :], ik=('out', 4 * g + j))
                    for half in range(2):
                        k.tt('dve', xb[:, 512 * half:512 * (half + 1)], xb[:, 512 * half:512 * (half + 1)],
                             pb[2 * j + half][:], ALU.add)
                    k.dma('sp', out_d[t0 + 128 * j: t0 + 128 * (j + 1), :], xb[:], ok=('out', 4 * g + j))
        k.barrier()
        build.stats = (k.nins, k.nwaits)
    return nc


def prep_shared(inp, L=2):
    f = np.float32
    w_in = np.asarray(inp['w_in'], f)
    win = np.zeros((L, NB_IN, 128, 8, 128), f)
    for bi_, (nm, idx, col0, ncols) in enumerate(IN_BLOCKS):
        win[:, bi_, :, :, :ncols] = w_in[:, :, col0:col0 + ncols].reshape(L, 8, 128, ncols).transpose(0, 2, 1, 3)
    w_out = np.asarray(inp['w_out'], f)
    wout = np.zeros((L, 12, 128, 1024), f)
    for h in range(4):
        wout[:, h, :96] = w_out[:, 96 * h:96 * (h + 1)]
    for h in range(6):
        wout[:, 4 + h, :64] = w_out[:, 384 + 64 * h:384 + 64 * (h + 1)]
    for c in range(2):
        wout[:, 10 + c] = w_out[:, 768 + 128 * c:768 + 128 * (c + 1)]
    w_up = np.asarray(inp['ffn_w_up'], f)
    wup = np.zeros((L, NB_UP, 128, 8, 128), f)
    for b in range(NKD):
        wup[:, 2 * b] = w_up[:, :, 128 * b:128 * (b + 1)].reshape(L, 8, 128, 128).transpose(0, 2, 1, 3)
        wup[:, 2 * b + 1] = w_up[:, :, DFF + 128 * b:DFF + 128 * (b + 1)].reshape(L, 8, 128, 128).transpose(0, 2, 1, 3)
    wdn = np.ascontiguousarray(np.asarray(inp['ffn_w_down'], f).reshape(L, NKD, 128, 1024))
    pp128 = np.zeros((128, L, PC_N), f)
    pp96 = np.zeros((96, L, Q_N), f)
    pp64 = np.zeros((64, L, 6), f)
    s5pad = np.zeros((L, 7, 128, 8, 128), f)
    for l in range(L):
        pp128[:, l, PC_GMIX:PC_GMIX + 8] = np.asarray(inp['norm_mix_g'], f)[l].reshape(8, 128).T
        pp128[:, l, PC_GFFN:PC_GFFN + 8] = np.asarray(inp['norm_ffn_g'], f)[l].reshape(8, 128).T
        pp128[:, l, PC_SBQG] = np.tile(np.asarray(inp['sb_q_g'], f)[l], 2)
        pp128[:, l, PC_SBKG] = np.tile(np.asarray(inp['sb_k_g'], f)[l], 2)
        pp128[:, l, PC_GLUB:PC_GLUB + 2] = np.asarray(inp['s5_glu_b'], f)[l].reshape(2, 128).T
        pp128[:, l, PC_S5G:PC_S5G + 2] = np.asarray(inp['s5_out_g'], f)[l].reshape(2, 128).T
        pp128[:, l, PC_S5D:PC_S5D + 2] = np.asarray(inp['s5_d'], f)[l].reshape(2, 128).T
        fcw = np.asarray(inp['ffn_conv_w'], f)[l]
        pp128[:, l, PC_FCW:PC_FCW + 66] = fcw.reshape(3, NKD, 128).transpose(2, 1, 0).reshape(128, 66)
        pp128[:, l, PC_GATEB:PC_GATEB + 8] = np.asarray(inp['ml_gate_b'], f)[l][None, :]
        are = np.asarray(inp['s5_a_re'], f)[l]; aim = np.asarray(inp['s5_a_im'], f)[l]
        ldt = np.repeat(np.asarray(inp['s5_log_dt'], f)[l][:, None], 64, axis=1)
        pp128[:, l, PC_ARE:PC_ARE + 8] = are.reshape(8, 128).T
        pp128[:, l, PC_AIM:PC_AIM + 8] = aim.reshape(8, 128).T
        pp128[:, l, PC_LDT:PC_LDT + 8] = ldt.reshape(8, 128).T
        cw = np.asarray(inp['ml_conv_w'], f)[l]
        pp96[:, l, Q_CW:Q_CW + 32] = cw.reshape(4, 8, 96).transpose(2, 1, 0).reshape(96, 32)
        pp96[:, l, Q_CB:Q_CB + 8] = np.asarray(inp['ml_conv_b'], f)[l].reshape(8, 96).T
        pp96[:, l, Q_OG:Q_OG + 4] = np.asarray(inp['ml_out_g'], f)[l].T
        pp64[:, l, :] = np.asarray(inp['sb_out_g'], f)[l].T
        s5pad[l, 0] = are.reshape(1, 8, 128)
        s5pad[l, 1] = aim.reshape(1, 8, 128)
        s5pad[l, 2] = ldt.reshape(1, 8, 128)
        bre = np.asarray(inp['s5_b_re'], f)[l]; bim = np.asarray(inp['s5_b_im'], f)[l]
        cre = np.asarray(inp['s5_c_re'], f)[l]; cim = np.asarray(inp['s5_c_im'], f)[l]
        for gq in range(16):
            i, gg = gq // 2, gq % 2
            r0 = 32 * (i % 4) + 16 * gg
            s5pad[l, 3, r0:r0 + 16, i, 64 * gg:64 * gg + 64] = bre[gq].T
            s5pad[l, 4, r0:r0 + 16, i, 64 * gg:64 * gg + 64] = bim[gq].T
            s5pad[l, 5, 64 * gg:64 * gg + 64, i, r0:r0 + 16] = cre[gq].T
            s5pad[l, 6, 64 * gg:64 * gg + 64, i, r0:r0 + 16] = cim[gq].T
    gluw = np.ascontiguousarray(np.asarray(inp['s5_glu_w'], f).reshape(L, 2, 128, 256).transpose(0, 2, 1, 3))
    return {
        'win': win.reshape(L, NB_IN, 128, 1024), 'wout': wout, 'wup': wup.reshape(L, NB_UP, 128, 1024), 'wdn': wdn,
        'pp128': pp128, 'pp96': pp96, 'pp64': pp64, 's5pad': s5pad.reshape(L, 7, 128, 1024), 'gluw': gluw,
    }


_CACHE = {}


def kernel(**inputs):
    x = np.asarray(inputs['x'], np.float32)
    B, T, _ = x.shape
    L = np.asarray(inputs['w_in']).shape[0]
    key = (T, L)
    if key not in _CACHE:
        _CACHE[key] = build(T=T, L=L)
    nc = _CACHE[key]
    shared = prep_shared(inputs, L)
    in_maps = []
    for b in range(B):
        m = dict(shared)
        m['x'] = np.ascontiguousarray(x[b])
        in_maps.append(m)
    res = run_bass_kernel_spmd(nc, in_maps, core_ids=list(range(B)))
    return np.stack([np.asarray(r['out'], np.float32) for r in res.results], axis=0)
```
